# Optimizing a Trainium2 kernel written in Bass

```python
import math
import jax, jax.numpy as jnp
from jax import lax
import numpy as np

D_MODEL = 1024
BATCH = 16
SEQ = 2048
DEPTH = 2

HEAD_DIM = 64
BRANCH_W = 512
N_BRANCH = 3
MOBA_HEADS = BRANCH_W // HEAD_DIM
MOBA_BLOCK = 256
MOBA_TOPK = 3
RWKV_HEADS = BRANCH_W // HEAD_DIM
RWKV_HEAD = HEAD_DIM
LORA_DECAY = 64
LORA_AAA = 64
LORA_GATE = 160
LORA_MV = 32
RWKV_GN_EPS = 64e-5
DSA_HEADS = BRANCH_W // HEAD_DIM
DSA_KV_RANK = 256
IDX_HEADS = 8
IDX_DIM = 32
DSA_TOPK_MAX = 256
REL_BUCKETS = 32
REL_MAX_DIST = 128
D_FF = ((8 * D_MODEL + 3 * 256 - 1) // (3 * 256)) * 256
NORM_EPS = 1e-6
Q_BLOCK = 16

RWKV_SPLITS = (BRANCH_W, LORA_DECAY, BRANCH_W, BRANCH_W, LORA_AAA, LORA_GATE)
DSA_SPLITS = (BRANCH_W, DSA_KV_RANK, IDX_HEADS * IDX_DIM, IDX_DIM, IDX_HEADS)
RWKV_IN = sum(RWKV_SPLITS)
DSA_IN = sum(DSA_SPLITS)
REGION_SPLITS = (3 * BRANCH_W, RWKV_IN, DSA_IN, N_BRANCH * D_MODEL)
D_IN = sum(REGION_SPLITS)

kernel_name = 'hybrid_moba_rwkv7_dsa_gated_block'


def _split(t, widths):
    idx = np.cumsum(widths)[:-1].tolist()
    return jnp.split(t, idx, axis=-1)


def rms_norm(x, g):
    xf = x.astype(jnp.float32)
    y = xf * lax.rsqrt(jnp.mean(xf * xf, axis=-1, keepdims=True) + NORM_EPS)
    return (y * g.astype(jnp.float32)).astype(x.dtype)


def token_shift(t):
    return jnp.pad(t, ((0, 0), (1, 0), (0, 0)))[:, :-1]


def rel_bucket(dist):
    n = jnp.maximum(dist, 0)
    max_exact = REL_BUCKETS // 2
    nf = jnp.maximum(n, 1).astype(jnp.float32)
    large = max_exact + (jnp.log(nf / max_exact) / math.log(REL_MAX_DIST / max_exact)
                         * (REL_BUCKETS - max_exact)).astype(jnp.int32)
    large = jnp.minimum(large, REL_BUCKETS - 1)
    return jnp.where(n < max_exact, n, large)


def moba_attention(q, k, v, rel_tab):
    B, S, H, Dh = q.shape
    f32 = jnp.float32
    nb = -(-S // MOBA_BLOCK)
    sp = nb * MOBA_BLOCK
    pad = ((0, 0), (0, sp - S), (0, 0), (0, 0))
    qh = jnp.transpose(q, (0, 2, 1, 3))
    kb = jnp.pad(k, pad).transpose(0, 2, 1, 3).reshape(B, H, nb, MOBA_BLOCK, Dh)
    vb = jnp.pad(v, pad).transpose(0, 2, 1, 3).reshape(B, H, nb, MOBA_BLOCK, Dh)
    scale = Dh ** -0.5
    tab_t = rel_tab.T
    n_sel = min(MOBA_TOPK, nb - 1)
    own = jnp.arange(S) // MOBA_BLOCK
    h_idx = jnp.arange(H)[:, None, None]
    b_idx = jnp.arange(B)[:, None, None, None]
    hb_idx = jnp.arange(H)[None, :, None, None]
    if n_sel > 0:
        kmean = jnp.mean(kb.astype(f32), axis=3)
        gate = jnp.einsum('bhsd,bhnd->bhsn', qh.astype(f32), kmean)
        past = jnp.arange(nb)[None, :] < own[:, None]
        gate = jnp.where(past, gate, -jnp.inf)
        _, sel = lax.top_k(gate, n_sel)
        sel_valid = jnp.arange(n_sel)[None, :] < own[:, None]

    def step(c):
        t0 = c * Q_BLOCK
        tq = t0 + jnp.arange(Q_BLOCK)
        qc = lax.dynamic_slice_in_dim(qh, t0, Q_BLOCK, axis=2)
        blk = t0 // MOBA_BLOCK
        k_own = lax.dynamic_index_in_dim(kb, blk, axis=2, keepdims=False)
        v_own = lax.dynamic_index_in_dim(vb, blk, axis=2, keepdims=False)
        dist_own = tq[:, None] - (blk * MOBA_BLOCK + jnp.arange(MOBA_BLOCK))[None, :]
        lo = jnp.einsum('bhqd,bhkd->bhqk', qc, k_own, preferred_element_type=f32) * scale
        lo = jnp.where(dist_own >= 0, lo + tab_t[:, rel_bucket(dist_own)], -jnp.inf)
        if n_sel > 0:
            sc = lax.dynamic_slice_in_dim(sel, t0, Q_BLOCK, axis=2)
            valid = lax.dynamic_slice_in_dim(sel_valid, t0, Q_BLOCK, axis=0)
            npk = n_sel * MOBA_BLOCK
            kg = kb[b_idx, hb_idx, sc].reshape(B, H, Q_BLOCK, npk, Dh)
            vg = vb[b_idx, hb_idx, sc].reshape(B, H, Q_BLOCK, npk, Dh)
            kpos = (sc[..., None] * MOBA_BLOCK + jnp.arange(MOBA_BLOCK)).reshape(B, H, Q_BLOCK, npk)
            dist = tq[None, None, :, None] - kpos
            lp = jnp.einsum('bhqd,bhqkd->bhqk', qc, kg, preferred_element_type=f32) * scale
            lp = lp + tab_t[h_idx, rel_bucket(dist)]
            validk = jnp.repeat(valid, MOBA_BLOCK, axis=1)
            lp = jnp.where(validk[None, None], lp, -jnp.inf)
            p = jax.nn.softmax(jnp.concatenate([lp, lo], axis=-1), axis=-1).astype(v.dtype)
            out = (jnp.einsum('bhqk,bhqkd->bhqd', p[..., :npk], vg)
                   + jnp.einsum('bhqk,bhkd->bhqd', p[..., npk:], v_own))
        else:
            p = jax.nn.softmax(lo, axis=-1).astype(v.dtype)
            out = jnp.einsum('bhqk,bhkd->bhqd', p, v_own)
        return out

    outs = lax.map(step, jnp.arange(S // Q_BLOCK))
    return outs.transpose(1, 0, 3, 2, 4).reshape(B, S, H * Dh)


def dsa_attention(q, k, v, q_idx, k_idx, w_idx, rel_tab):
    B, S, H, Dh = q.shape
    f32 = jnp.float32
    n_keep = min(DSA_TOPK_MAX, S // 4)
    scale = Dh ** -0.5
    b_idx = jnp.arange(B)[:, None, None]
    key_pos = jnp.arange(S)

    def step(c):
        t0 = c * Q_BLOCK
        tq = t0 + jnp.arange(Q_BLOCK)
        qi = lax.dynamic_slice_in_dim(q_idx, t0, Q_BLOCK, axis=1)
        wi = lax.dynamic_slice_in_dim(w_idx, t0, Q_BLOCK, axis=1).astype(f32)
        isc = jnp.einsum('bqhd,bsd->bqhs', qi, k_idx, preferred_element_type=f32)
        isc = jnp.einsum('bqhs,bqh->bqs', jax.nn.relu(isc), wi)
        isc = jnp.where(key_pos[None, None, :] <= tq[None, :, None], isc, -jnp.inf)
        _, sel = lax.top_k(isc, n_keep)
        valid = jnp.arange(n_keep)[None, :] <= tq[:, None]
        kg = k[b_idx, sel]
        vg = v[b_idx, sel]
        qc = lax.dynamic_slice_in_dim(q, t0, Q_BLOCK, axis=1)
        lg = jnp.einsum('bqhd,bqkhd->bqhk', qc, kg, preferred_element_type=f32) * scale
        bias = rel_tab[rel_bucket(tq[None, :, None] - sel)]
        lg = jnp.where(valid[None, :, None, :], lg + jnp.moveaxis(bias, -1, 2), -jnp.inf)
        p = jax.nn.softmax(lg, axis=-1).astype(v.dtype)
        return jnp.einsum('bqhk,bqkhd->bqhd', p, vg)

    outs = lax.map(step, jnp.arange(S // Q_BLOCK))
    return outs.transpose(1, 0, 2, 3, 4).reshape(B, S, H * Dh)


def wkv7_scan(r, decay, k, v, kk, a):
    B, S, H, N = r.shape

    def step(state, inp):
        r_t, w_t, k_t, v_t, kk_t, a_t = inp
        sa = jnp.einsum('bhvk,bhk->bhv', state, -kk_t)
        state = (state * w_t[:, :, None, :] + sa[..., None] * (kk_t * a_t)[:, :, None, :]
                 + v_t[..., None] * k_t[:, :, None, :])
        return state, jnp.einsum('bhvk,bhk->bhv', state, r_t)

    xs = tuple(jnp.moveaxis(t, 1, 0) for t in (r, decay, k, v, kk, a))
    _, out = lax.scan(step, jnp.zeros((B, H, N, N), jnp.float32), xs)
    return jnp.moveaxis(out, 0, 1)


def rwkv7_time_mix(p, mu, w0, w2, a0, a2, g2, k_k, k_a, r_k, ln_w, ln_b, v_first, vres):
    B, S, _ = p.shape
    p = p.astype(jnp.float32)
    p = p + (token_shift(p) - p) * mu
    r, wd, k, v, ad, gd = _split(p, RWKV_SPLITS)
    w = -jax.nn.softplus(-(w0 + jnp.tanh(wd) @ w2)) - 0.5
    decay = jnp.exp(-jnp.exp(w))
    a = jax.nn.sigmoid(a0 + ad @ a2)
    g = jax.nn.sigmoid(gd) @ g2
    if vres is None:
        v_first = v
    else:
        v0, va, vb = vres
        v = v + (v_first - v) * jax.nn.sigmoid(v0 + (v @ va) @ vb)
    hs = lambda t: t.reshape(B, S, RWKV_HEADS, RWKV_HEAD)
    kk = hs(k * k_k)
    kk = kk / jnp.maximum(jnp.sqrt(jnp.sum(kk * kk, axis=-1, keepdims=True)), 1e-12)
    k = k * (1.0 + (a - 1.0) * k_a)
    r, k, v, a, decay = hs(r), hs(k), hs(v), hs(a), hs(decay)
    o = wkv7_scan(r, decay, k, v, kk, a)
    mean = jnp.mean(o, axis=-1, keepdims=True)
    var = jnp.mean(jnp.square(o - mean), axis=-1, keepdims=True)
    gn_w = ln_w.astype(jnp.float32).reshape(RWKV_HEADS, RWKV_HEAD)
    gn_b = ln_b.astype(jnp.float32).reshape(RWKV_HEADS, RWKV_HEAD)
    o = (o - mean) * lax.rsqrt(var + RWKV_GN_EPS) * gn_w + gn_b
    o = o + jnp.sum(r * k * r_k, axis=-1, keepdims=True) * v
    return o.reshape(B, S, BRANCH_W) * g, v_first


def setup_inputs(seed: int = 0) -> dict:
    key = jax.random.key(seed)
    ks = jax.random.split(key, 26)
    L = DEPTH
    nrm = lambda k, shape, s: s * jax.random.normal(k, shape, jnp.float32)
    return {
        'x': nrm(ks[0], (BATCH, SEQ, D_MODEL), 1.0),
        'w_in': nrm(ks[1], (L, D_MODEL, D_IN), D_MODEL ** -0.5),
        'norm_mix': 1.0 + nrm(ks[2], (L, D_MODEL), 0.05),
        'norm_ffn': 1.0 + nrm(ks[3], (L, D_MODEL), 0.05),
        'qk_norm': 1.0 + nrm(ks[4], (L, 4, HEAD_DIM), 0.05),
        'rel_bias': nrm(ks[5], (REL_BUCKETS, MOBA_HEADS + DSA_HEADS), 0.5),
        'rwkv_mu': jax.random.uniform(ks[6], (L, RWKV_IN), jnp.float32),
        'rwkv_w0': jnp.linspace(-6.0, -1.0, BRANCH_W, dtype=jnp.float32)[None, :] + nrm(ks[7], (L, BRANCH_W), 0.1),
        'rwkv_w2': nrm(ks[8], (L, LORA_DECAY, BRANCH_W), 0.5 * LORA_DECAY ** -0.5),
        'rwkv_a0': nrm(ks[9], (L, BRANCH_W), 0.1),
        'rwkv_a2': nrm(ks[10], (L, LORA_AAA, BRANCH_W), LORA_AAA ** -0.5),
        'rwkv_g2': nrm(ks[11], (L, LORA_GATE, BRANCH_W), LORA_GATE ** -0.5),
        'rwkv_kk': 0.85 + nrm(ks[12], (L, BRANCH_W), 0.05),
        'rwkv_ka': 1.0 + nrm(ks[13], (L, BRANCH_W), 0.05),
        'rwkv_rk': nrm(ks[14], (L, RWKV_HEADS, RWKV_HEAD), 0.1),
        'rwkv_ln_w': 1.0 + nrm(ks[15], (L, BRANCH_W), 0.05),
        'rwkv_ln_b': nrm(ks[16], (L, BRANCH_W), 0.02),
        'rwkv_v0': nrm(ks[17], (L - 1, BRANCH_W), 0.1),
        'rwkv_va': nrm(ks[18], (L - 1, BRANCH_W, LORA_MV), BRANCH_W ** -0.5),
        'rwkv_vb': nrm(ks[19], (L - 1, LORA_MV, BRANCH_W), LORA_MV ** -0.5),
        'dsa_kv_norm': 1.0 + nrm(ks[20], (L, DSA_KV_RANK), 0.05),
        'dsa_kv_up': nrm(ks[21], (L, DSA_KV_RANK, 2 * BRANCH_W), DSA_KV_RANK ** -0.5),
        'w_branch': nrm(ks[22], (L, N_BRANCH, BRANCH_W, D_MODEL), BRANCH_W ** -0.5),
        'w_o': nrm(ks[23], (L, D_MODEL, D_MODEL), D_MODEL ** -0.5),
        'w_ffn_in': nrm(ks[24], (L, D_MODEL, 2 * D_FF), D_MODEL ** -0.5),
        'w_ffn_out': nrm(ks[25], (L, D_FF, D_MODEL), D_FF ** -0.5),
    }


def reference(x, w_in, norm_mix, norm_ffn, qk_norm, rel_bias, rwkv_mu, rwkv_w0, rwkv_w2, rwkv_a0,
              rwkv_a2, rwkv_g2, rwkv_kk, rwkv_ka, rwkv_rk, rwkv_ln_w, rwkv_ln_b, rwkv_v0, rwkv_va,
              rwkv_vb, dsa_kv_norm, dsa_kv_up, w_branch, w_o, w_ffn_in, w_ffn_out):
    B, S, D = x.shape
    moba_tab = rel_bias[:, :MOBA_HEADS]
    dsa_tab = rel_bias[:, MOBA_HEADS:]
    heads = lambda t, n: t.reshape(B, S, n, HEAD_DIM)
    v_first = None
    for l in range(DEPTH):
        h = rms_norm(x, norm_mix[l])
        p = h @ w_in[l]
        p_moba, p_rwkv, p_dsa, p_gate = _split(p, REGION_SPLITS)
        mq, mk, mv = _split(p_moba, (BRANCH_W, BRANCH_W, BRANCH_W))
        mq = rms_norm(heads(mq, MOBA_HEADS), qk_norm[l, 0])
        mk = rms_norm(heads(mk, MOBA_HEADS), qk_norm[l, 1])
        o_moba = moba_attention(mq, mk, heads(mv, MOBA_HEADS), moba_tab)
        vres = None if l == 0 else (rwkv_v0[l - 1], rwkv_va[l - 1], rwkv_vb[l - 1])
        o_rwkv, v_first = rwkv7_time_mix(p_rwkv, rwkv_mu[l], rwkv_w0[l], rwkv_w2[l], rwkv_a0[l],
                                         rwkv_a2[l], rwkv_g2[l], rwkv_kk[l], rwkv_ka[l], rwkv_rk[l],
                                         rwkv_ln_w[l], rwkv_ln_b[l], v_first, vres)
        dq, dc, diq, dik, diw = _split(p_dsa, DSA_SPLITS)
        dk, dv = _split(rms_norm(dc, dsa_kv_norm[l]) @ dsa_kv_up[l], (BRANCH_W, BRANCH_W))
        dq = rms_norm(heads(dq, DSA_HEADS), qk_norm[l, 2])
        dk = rms_norm(heads(dk, DSA_HEADS), qk_norm[l, 3])
        o_dsa = dsa_attention(dq, dk, heads(dv, DSA_HEADS), diq.reshape(B, S, IDX_HEADS, IDX_DIM),
                              dik, diw, dsa_tab)
        o = jnp.stack([o_moba, o_rwkv.astype(x.dtype), o_dsa], axis=2)
        y = jnp.einsum('bsnc,ncd->bsnd', o, w_branch[l])
        gate = jax.nn.sigmoid(p_gate.reshape(B, S, N_BRANCH, D))
        x = x + jnp.sum(gate * y, axis=2) @ w_o[l]
        fg, fu = _split(rms_norm(x, norm_ffn[l]) @ w_ffn_in[l], (D_FF, D_FF))
        x = x + (jax.nn.silu(fg) * fu) @ w_ffn_out[l]
    return x
```

```python
import numpy as np
from contextlib import ExitStack
import concourse.bass as bass
import concourse.mybir as mybir

F32 = mybir.dt.float32
BF16 = mybir.dt.bfloat16
AF = mybir.ActivationFunctionType
ALU = mybir.AluOpType
AX = mybir.AxisListType


class Buf:
    __slots__ = ("name", "wr", "rd", "nowaw")

    def __init__(self, name=""):
        self.name = name
        self.wr = {}
        self.rd = {}
        self.nowaw = False


class V:
    __slots__ = ("ap", "bufs")

    def __init__(self, ap, bufs):
        self.ap = ap
        self.bufs = bufs


class Tile:
    def __init__(self, handle, name):
        self.h = handle
        self.name = name
        self.buf = Buf(name)
        self.subs = {}

    def __getitem__(self, idx):
        return V(self.h[idx], [self.buf])

    def sub(self, key):
        b = self.subs.get(key)
        if b is None:
            b = self.subs[key] = Buf(f"{self.name}/{key}")
        return _Sub(self, b)

    def subs_of(self, keys):
        return [self.sub(k).b for k in keys]


class _Sub:
    def __init__(self, t, b):
        self.t = t
        self.b = b

    def __getitem__(self, idx):
        return V(self.t.h[idx], [self.b])


class Op:
    __slots__ = ("eng", "fn", "deps", "signal", "sem", "sigval", "chan", "n")


ENGS = ("pe", "act", "dve", "pool", "sp")


class Sched:
    def __init__(self, nc):
        self.nc = nc
        self.ops = []
        self.chan_last = {}
        self.E = {"pe": nc.tensor, "act": nc.scalar, "dve": nc.vector, "pool": nc.gpsimd, "sp": nc.sync}

    def add(self, eng, fn, reads, writes, chan=None, n=1):
        idx = len(self.ops)
        key = eng if chan is None else "d:" + chan
        deps = set()
        for b in reads:
            deps.update(b.wr.values())
        for b in writes:
            if not b.nowaw:
                deps.update(b.wr.values())
            deps.update(b.rd.values())
        for b in reads:
            b.rd[key] = idx
        for b in writes:
            if b.rd:
                b.wr = {key: idx}
                b.rd = {}
            else:
                b.wr[key] = idx
        if chan is not None:
            if chan in self.chan_last:
                deps.add(self.chan_last[chan])
            self.chan_last[chan] = idx
        deps.discard(idx)
        best = {}
        for j in deps:
            o = self.ops[j]
            k2 = o.eng if o.chan is None else "d:" + o.chan
            if k2 == "pe" and eng == "pe" and chan is None:
                continue
            if k2 not in best or best[k2] < j:
                best[k2] = j
        op = Op()
        op.eng = eng
        op.fn = fn
        op.deps = sorted(best.values())
        op.signal = chan is not None
        op.sem = None
        op.sigval = None
        op.chan = chan
        op.n = n
        for j in op.deps:
            self.ops[j].signal = True
        self.ops.append(op)
        return idx

    def barrier(self):
        last = {}
        for idx, o in enumerate(self.ops):
            if o.fn is not None:
                last[o.eng if o.chan is None else "d:" + o.chan] = idx
        deps = sorted(set(last.values()))
        for e in ENGS:
            op = Op()
            op.eng = e
            op.fn = None
            op.deps = list(deps)
            op.signal = False
            op.sem = None
            op.sigval = None
            op.chan = None
            op.n = 0
            self.ops.append(op)
        for j in deps:
            self.ops[j].signal = True

    def emit(self, es):
        nc = self.nc
        sems = {e: es.enter_context(nc.semaphore("s_" + e)) for e in ENGS}
        chans = sorted(set(o.chan for o in self.ops if o.chan is not None))
        for c in chans:
            sems["d:" + c] = es.enter_context(nc.semaphore("d_" + c))
        cnt = {k: 0 for k in sems}
        seen = {e: {} for e in ENGS}
        nwait = 0
        for op in self.ops:
            E = self.E[op.eng]
            sn = seen[op.eng]
            for j in op.deps:
                d = self.ops[j]
                if sn.get(d.sem, 0) < d.sigval:
                    E.wait_ge(sems[d.sem], d.sigval)
                    sn[d.sem] = d.sigval
                    nwait += 1
            if op.fn is None:
                continue
            ins = op.fn(E)
            if op.chan is not None:
                sk = "d:" + op.chan
                if not isinstance(ins, (list, tuple)):
                    ins = [ins]
                assert len(ins) == op.n
                for i_ in ins:
                    i_.then_inc(sems[sk], 16)
                cnt[sk] += 16 * op.n
                op.sem = sk
                op.sigval = cnt[sk]
            elif op.signal:
                ins.then_inc(sems[op.eng], 1)
                cnt[op.eng] += 1
                op.sem = op.eng
                op.sigval = cnt[op.eng]
        sp = self.E["sp"]
        for k, v in cnt.items():
            if v > 0 and seen["sp"].get(k, 0) < v:
                sp.wait_ge(sems[k], v)
        return dict(n_ops=len(self.ops), n_wait=nwait, cnt=cnt)


def _bufs(*vs):
    out = []
    for v in vs:
        if isinstance(v, V):
            out.extend(v.bufs)
    return out


def _ap(v):
    return v.ap if isinstance(v, V) else v


class K:
    def __init__(self, nc, es):
        self.nc = nc
        self.es = es
        self.s = Sched(nc)
        self.psum = []
        self.ps_i = 0
        self.dq = 0

    def tile(self, shape, dtype, name, es=None):
        self.dq += 1
        name = f"{name}_{self.dq}"
        h = (es or self.es).enter_context(self.nc.sbuf_tensor(name, list(shape), dtype))
        return Tile(h, name)

    def ptile(self, shape, dtype, name, es=None):
        h = (es or self.es).enter_context(self.nc.psum_tensor(name, list(shape), dtype))
        return Tile(h, name)

    def dram(self, name, shape, dtype, kind="Internal"):
        h = self.nc.dram_tensor(name, list(shape), dtype, kind=kind)
        t = Tile(h.ap(), name)
        t.buf.nowaw = True
        return t

    def ps(self):
        t = self.psum[self.ps_i % len(self.psum)]
        self.ps_i += 1
        return t

    def mm(self, out, lhsT, rhs, start=True, stop=True, **kw):
        self.s.add("pe", lambda E: E.matmul(out.ap, lhsT.ap, rhs.ap, start=start, stop=stop, **kw),
                   _bufs(lhsT, rhs), _bufs(out))

    def tr(self, out, in_, ident):
        self.s.add("pe", lambda E: E.transpose(out.ap, in_.ap, ident.ap), _bufs(in_, ident), _bufs(out))

    def act(self, out, in_, func, bias=None, scale=None, accum=None):
        kw = {}
        if bias is not None:
            kw["bias"] = _ap(bias)
        if scale is not None:
            kw["scale"] = _ap(scale)
        if accum is not None:
            kw["accum_out"] = accum.ap
        self.s.add("act", lambda E: E.activation(out.ap, in_.ap, func, **kw),
                   _bufs(in_, bias, scale), _bufs(out, accum))

    def tt(self, eng, out, in0, in1, op):
        self.s.add(eng, lambda E: E.tensor_tensor(out.ap, in0.ap, in1.ap, op), _bufs(in0, in1), _bufs(out))

    def ts(self, eng, out, in0, s1, op0, s2=None, op1=None, accum=None):
        kw = {}
        if op1 is not None:
            kw["op1"] = op1
        if accum is not None:
            kw["accum_out"] = accum.ap
        self.s.add(eng, lambda E: E.tensor_scalar(out.ap, in0.ap, _ap(s1), _ap(s2), op0, **kw),
                   _bufs(in0, s1, s2), _bufs(out, accum))

    def stt(self, eng, out, in0, scalar, in1, op0, op1):
        self.s.add(eng, lambda E: E.scalar_tensor_tensor(out.ap, in0.ap, _ap(scalar), in1.ap, op0, op1),
                   _bufs(in0, scalar, in1), _bufs(out))

    def copy(self, eng, out, in_):
        if eng == "act":
            self.s.add("act", lambda E: E.copy(out.ap, in_.ap), _bufs(in_), _bufs(out))
        else:
            self.s.add(eng, lambda E: E.tensor_copy(out.ap, in_.ap), _bufs(in_), _bufs(out))

    def memset(self, eng, out, val):
        self.s.add(eng, lambda E: E.memset(out.ap, val), [], _bufs(out))

    def recip(self, out, in_):
        self.s.add("dve", lambda E: E.reciprocal(out.ap, in_.ap), _bufs(in_), _bufs(out))

    def reduce(self, out, in_, op, axis=AX.X):
        self.s.add("dve", lambda E: E.tensor_reduce(out.ap, in_.ap, axis, op), _bufs(in_), _bufs(out))

    def max8(self, out, in_):
        self.s.add("dve", lambda E: E.max(out.ap, in_.ap), _bufs(in_), _bufs(out))

    def match_replace(self, out, to_replace, in_values, imm):
        self.s.add("dve", lambda E: E.match_replace(out.ap, to_replace.ap, in_values.ap, imm),
                   _bufs(to_replace, in_values), _bufs(out))

    def scan(self, out, d0, d1, initial, op0, op1):
        self.s.add("dve", lambda E: E.tensor_tensor_scan(out.ap, d0.ap, d1.ap, _ap(initial), op0, op1),
                   _bufs(d0, d1, initial), _bufs(out))

    def dma(self, q, out, in_, chan, **kw):
        pairs = list(zip(out, in_)) if isinstance(out, (list, tuple)) else [(out, in_)]
        rb = []
        wb = []
        for o, i in pairs:
            rb += _bufs(i)
            wb += _bufs(o)

        def fn(E):
            return [E.dma_start(out=o.ap, in_=i.ap, **kw) for o, i in pairs]
        self.s.add(q, fn, rb, wb, chan=chan, n=len(pairs))

    def emit(self):
        return self.s.emit(self.es)
from concourse.bass_utils import run_bass_kernel_spmd

NCORE = 8
S = 2048
NB = 2
T = NB * S
D = 1024
DIN = 7496
DFF = 2816
C_MQ, C_MK, C_MV = 0, 512, 1024
C_RW = 1536
RW_ROLES = dict(r=(0, 512), wd=(512, 64), k=(576, 512), v=(1088, 512), ad=(1600, 64), gd=(1664, 160))
C_DQ, C_DC, C_DIQ, C_DIK, C_DIW = 3360, 3872, 4128, 4384, 4416
C_GATE = 4424
NEG = -30000.0


def rel_bucket_np(dist):
    n = np.maximum(dist, 0)
    nf = np.maximum(n, 1).astype(np.float32)
    large = 16 + (np.log(nf / np.float32(16)) / np.float32(np.log(128 / 16)) * np.float32(16)).astype(np.int32)
    large = np.minimum(large, 31)
    return np.where(n < 16, n, large)


def pvec_keys():
    keys = []
    for j in range(4):
        keys.append(("qkg", j))
    for role, (off, n) in RW_ROLES.items():
        for c in range((n + 127) // 128):
            keys.append(("mu", role, c))
    for nm in ("w0", "a0", "kk", "ka", "rk", "v0"):
        for c in range(4):
            keys.append((nm, c))
    for c in range(2):
        keys.append(("kvn", c))
    return keys


PV_KEYS = pvec_keys()
PV_IDX = {k_: i for i, k_ in enumerate(PV_KEYS)}


def pvec_host(inp, l):
    out = np.zeros((128, len(PV_KEYS)), np.float32)
    for i, key in enumerate(PV_KEYS):
        if key[0] == "qkg":
            v = np.tile(inp["qk_norm"][l, key[1]], 2)
        elif key[0] == "mu":
            off, n = RW_ROLES[key[1]]
            c = key[2]
            v = inp["rwkv_mu"][l, off + c * 128: off + min(n, (c + 1) * 128)]
        elif key[0] == "kvn":
            v = inp["dsa_kv_norm"][l, key[1] * 128:(key[1] + 1) * 128]
        elif key[0] == "v0":
            v = inp["rwkv_v0"][0, key[1] * 128:(key[1] + 1) * 128]
        else:
            nm = dict(w0="rwkv_w0", a0="rwkv_a0", kk="rwkv_kk", ka="rwkv_ka", rk="rwkv_rk")[key[0]]
            v = inp[nm][l].reshape(512)[key[1] * 128:(key[1] + 1) * 128]
        out[:len(v), i] = v
    return out


def host_consts(inp):
    c = {}
    c["ident"] = np.eye(128, dtype=np.float32)
    b = np.zeros((128, 128), np.float32)
    b[:64, :64] = 1.0 / 64
    b[64:, 64:] = 1.0 / 64
    c["blk64"] = b
    c["pvec"] = np.stack([pvec_host(inp, l) for l in range(2)])
    kk_, qq_ = np.meshgrid(np.arange(128), np.arange(128), indexing="ij")
    bd = rel_bucket_np(qq_ - kk_)
    bn = rel_bucket_np(128 + qq_ - kk_)
    rb = inp["rel_bias"]
    relb = np.zeros((16, 2, 128, 128), np.float32)
    for h in range(16):
        relb[h, 0] = rb[bd, h]
        relb[h, 1] = rb[bn, h]
    c["relb"] = relb
    c["caus"] = np.where(qq_ >= kk_, 0.0, NEG).astype(np.float32)
    c["relfar"] = np.tile(rb[31:32, :], (128, 1)).astype(np.float32)
    c["causq"] = np.where(kk_ >= qq_, 0.0, -1e30).astype(np.float32)
    su = np.zeros((64, 3, 64), np.float32)
    ii, tt_ = np.meshgrid(np.arange(64), np.arange(64), indexing="ij")
    su[:, 0, :] = (ii < tt_)
    su[:, 1, :] = (ii <= tt_)
    su[:, 2, :] = (ii > tt_)
    c["umask"] = su
    return c


class Rot:
    def __init__(self, k, shape, dtype, name, n, es=None, init=None):
        self.tiles = [k.tile(shape, dtype, f"{name}{i}", es=es) for i in range(n)]
        self.i = 0
        if init is not None:
            for t in self.tiles:
                k.memset("dve", t[:], init)

    def next(self):
        t = self.tiles[self.i % len(self.tiles)]
        self.i += 1
        return t


def dv(t, ap, keys=None):
    return V(ap, [t.buf] if keys is None else t.subs_of(keys))


def build(dbg=None, phases=("P", "A", "R", "X", "M", "F"), layers=(0, 1), lim=None):
    dbg = dbg or {}
    lim = lim or {}
    nc = bass.Bass("TRN2", target_bir_lowering=False)
    es = ExitStack()
    with es:
        k = K(nc, es)

        def dram(name, shape, dtype, kind=None):
            return k.dram(name, shape, dtype, kind=kind or dbg.get(name, "Internal"))
        EI = "ExternalInput"
        x_in = dram("x", [T, D], F32, EI) if ("P" in phases or "M" in phases) else None
        w_in = dram("w_in", [2, D, DIN], F32, EI) if "P" in phases else None
        norm_mix = dram("norm_mix", [2, D], F32, EI)
        norm_ffn = dram("norm_ffn", [2, D], F32, EI)
        kv_up = dram("dsa_kv_up", [2, 256, 1024], F32, EI) if "P" in phases else None
        w_branch = dram("w_branch", [2, 3, 512, D], F32, EI) if "M" in phases else None
        w_o = dram("w_o", [2, D, D], F32, EI) if "M" in phases else None
        w_ffn_in = dram("w_ffn_in", [2, D, 2 * DFF], F32, EI) if "F" in phases else None
        w_ffn_out = dram("w_ffn_out", [2, DFF, D], F32, EI) if "F" in phases else None
        ident_d = dram("ident", [128, 128], F32, EI)
        blk64_d = dram("blk64", [128, 128], F32, EI)
        pvec_d = dram("pvec", [2, 128, len(PV_KEYS)], F32, EI)
        relb_d = dram("relb", [16, 2, 128, 128], F32, EI)
        caus_d = dram("caus", [128, 128], F32, EI)
        relfar_d = dram("relfar", [128, 16], F32, EI)
        causq_d = dram("causq", [128, 128], F32, EI)
        umask_d = dram("umask", [64, 3, 64], F32, EI)
        if "R" in phases:
            rw_w2 = dram("rwkv_w2", [2, 64, 512], F32, EI)
            rw_a2 = dram("rwkv_a2", [2, 64, 512], F32, EI)
            rw_g2 = dram("rwkv_g2", [2, 160, 512], F32, EI)
            rw_va = dram("rwkv_va", [1, 512, 32], F32, EI)
            rw_vb = dram("rwkv_vb", [1, 32, 512], F32, EI)
            rw_lnw = dram("rwkv_ln_w", [2, 512], F32, EI)
            rw_lnb = dram("rwkv_ln_b", [2, 512], F32, EI)
        y_out = dram("y", [T, D], F32, "ExternalOutput") if "F" in phases else None

        qkT = {nm: dram(nm, [4, 128, T], BF16) for nm in ("mqT", "mkT", "dqT", "dkT")}
        vE = {nm: dram(nm, [T, 8 * 65], BF16) for nm in ("mvE", "dvE")}
        rwT = {nm: dram("rw_" + nm, [4, 128, T], F32) for nm in ("r", "k", "v")}
        wdT = dram("rw_wd", [64, T], F32)
        adT = dram("rw_ad", [64, T], F32)
        gsT = dram("rw_gs", [2, 128, T], F32)
        qiT = dram("qiT", [2, 128, T], F32)
        kiT = dram("kiT", [128, T], F32)
        wiD = dram("wiD", [128, T // 128, 8], F32)
        sgT = dram("sgT", [24, 128, T], BF16)
        oT = {nm: dram("oT_" + nm, [4, 128, T], BF16) for nm in ("m", "r", "d")}
        vfirst = dram("vfirst", [4, 128, T], F32)
        xm_d = dram("xm", [T, D], F32)
        x1_d = dram("x1", [T, D], F32)

        for i in range(4):
            k.psum.append(k.ptile([128, 512], F32, f"ps{i}"))
        psR_t = [k.ptile([128, 512], F32, f"psR{i}") for i in range(4)]
        psR_i = [0]

        def psR():
            psR_i[0] += 1
            return psR_t[psR_i[0] % 4]

        idf = k.tile([128, 128], F32, "idf")
        idb = k.tile([128, 128], BF16, "idb")
        blkf = k.tile([128, 128], F32, "blkf")
        blkb = k.tile([128, 128], BF16, "blkb")
        onesb = k.tile([128, 128], BF16, "onesb")
        eps6 = k.tile([128, 1], F32, "eps6")
        pv = k.tile([128, 2, len(PV_KEYS)], F32, "pv")
        k.dma("sp", idf[:], ident_d[:], "c0")
        k.dma("sp", blkf[:], blk64_d[:], "c1")
        k.dma("sp", pv[:], dv(pvec_d, pvec_d.h.rearrange("l p n -> p l n")), "c2")
        k.copy("dve", idb[:], idf[:])
        k.copy("dve", blkb[:], blkf[:])
        k.memset("dve", onesb[:], 1.0 / 256)
        k.memset("dve", eps6[:], 1e-6)

        def pvc(l, *key, m=128):
            i = PV_IDX[tuple(key)]
            return pv[0:m, l, i:i + 1]

        def pbf(p):
            return V(p.h[:].bitcast(BF16), [p.buf])

        def norm_transpose(pes_outer, src, gain_row, hT, tok0, ntile, tag):
          with ExitStack() as pes:
            gb = k.tile([128, D], F32, "gb" + tag, es=pes)
            k.dma("sp", gb[:], dv(gain_row[0], gain_row[1].partition_broadcast(128)), "c3")
            xr = Rot(k, [128, D], F32, "xr" + tag, 2, es=pes)
            sqr = Rot(k, [128, D], BF16, "sqr" + tag, 2, es=pes)
            ssr = Rot(k, [128, 1], F32, "ssr" + tag, 2, es=pes)
            hbr = Rot(k, [128, D], BF16, "hbr" + tag, 2, es=pes)
            for t in range(ntile):
                xt, sq, ss, hb = xr.next(), sqr.next(), ssr.next(), hbr.next()
                r0 = tok0 + t * 128
                k.dma("sp", xt[:], src[r0:r0 + 128, :], f"xl{t % 2}")
                k.act(sq[:], xt[:], AF.Square, accum=ss[:])
                k.act(ss[:], ss[:], AF.Sqrt, bias=eps6[:], scale=1.0 / D)
                k.recip(ss[:], ss[:])
                k.stt("dve", hb[:], xt[:], ss[:], gb[:], ALU.mult, ALU.mult)
                p = k.ps()
                pb = pbf(p)
                for kc in range(8):
                    k.tr(V(pb.ap[:, kc * 128:(kc + 1) * 128], pb.bufs), hb[:, kc * 128:(kc + 1) * 128], idb[:])
                k.copy("act", hT[:, :, t * 128:(t + 1) * 128],
                       V(pb.ap[:, 0:1024].rearrange("p (k t) -> p k t", k=8), pb.bufs))
          k.s.barrier()

        wq = [0]

        stage = [None]

        def load_any(dst_v, src_v, nk, n):
            wq[0] += 1
            stg = stage[0].next()
            k.dma("sp", stg[:, 0:nk, 0:n], src_v, f"w{wq[0] % 2}")
            k.copy("act" if wq[0] % 2 else "dve", dst_v, stg[:, 0:nk, 0:n])

        def load_w(wt, src_ap, src_t, nk, n):
            load_any(wt[:, 0:nk, 0:n], dv(src_t, src_ap.rearrange("(kc p) c -> p kc c", p=128)), nk, n)

        def gemm_fm(wt, c0, n, hT, t0, nk):
            p = k.ps()
            for kc in range(nk):
                k.mm(p[0:n, :], wt[:, kc, c0:c0 + n], hT[:, kc, t0:t0 + 512], start=(kc == 0), stop=(kc == nk - 1))
            return p

        def gemm_tm(wt, n, hT, t0, nk):
            p = k.ps()
            for kc in range(nk):
                k.mm(p[:, 0:n], hT[:, kc, t0:t0 + 128], wt[:, kc, 0:n], start=(kc == 0), stop=(kc == nk - 1))
            return p

        for l in layers:
            xsrc = x_in if l == 0 else x1_d
            xdst = x1_d if l == 0 else y_out
            if "P" in phases:
                k.s.barrier()
                with ExitStack() as pes:
                    hT = k.tile([128, 8, T], BF16, "hT", es=pes)
                    cnT = k.tile([128, 2, T], BF16, "cnT", es=pes)
                    norm_transpose(pes, xsrc, (norm_mix, norm_mix.h[l:l + 1, :]), hT, 0, T // 128, "p")
                    wr = Rot(k, [128, 8, 512], BF16, "wt", 2, es=pes)
                    stage[0] = Rot(k, [128, 8, 512], F32, "stg", 1, es=pes)
                    qfr = Rot(k, [128, 512], F32, "qf", 3, es=pes)
                    sqr = Rot(k, [128, 512], BF16, "sq", 3, es=pes)
                    sdr = Rot(k, [128, 512], F32, "sd", 3, es=pes)
                    obr = Rot(k, [128, 512], BF16, "ob", 3, es=pes)
                    ver = Rot(k, [128, 8, 65], BF16, "ve", 3, es=pes, init=1.0)
                    ofr = Rot(k, [128, 512], F32, "of", 3, es=pes)
                    oq = [0]

                    def dout(dst_v, src_v):
                        oq[0] += 1
                        k.dma("sp", dst_v, src_v, f"o{oq[0] % 4}")

                    def qk_fm(wt, hsrc, nk, gkey, dst):
                        for c in range(4):
                            for tb in range(T // 512):
                                p = gemm_fm(wt, c * 128, 128, hsrc, tb * 512, nk)
                                qf, sq, sd, ob = qfr.next(), sqr.next(), sdr.next(), obr.next()
                                k.act(qf[:], p[:], AF.Copy)
                                k.act(sq[:], p[:], AF.Square)
                                p2 = k.ps()
                                k.mm(p2[:], blkb[:], sq[:])
                                k.act(sd[:], p2[:], AF.Sqrt, bias=eps6[:])
                                k.recip(sd[:], sd[:])
                                k.stt("dve", ob[:], qf[:], pvc(l, "qkg", gkey), sd[:], ALU.mult, ALU.mult)
                                dout(dv(dst, dst.h[c, :, tb * 512:(tb + 1) * 512]), ob[:])

                    def v_tm(wt, hsrc, nk, dst):
                        for tt in range(T // 128):
                            p = gemm_tm(wt, 512, hsrc, tt * 128, nk)
                            ve = ver.next()
                            k.copy("act" if tt % 2 else "dve", ve[:, :, 0:64],
                                   V(p.h[:, :].rearrange("p (h d) -> p h d", h=8), [p.buf]))
                            dout(dv(dst, dst.h[tt * 128:(tt + 1) * 128, :]),
                                 V(ve.h[:].rearrange("p h d -> p (h d)"), [ve.buf]))

                    for (c0, gk, dst) in ((C_MQ, 0, qkT["mqT"]), (C_MK, 1, qkT["mkT"]), (C_DQ, 2, qkT["dqT"])):
                        wt = wr.next()
                        load_w(wt, w_in.h[l, :, c0:c0 + 512], w_in, 8, 512)
                        qk_fm(wt, hT, 8, gk, dst)
                    wt = wr.next()
                    load_w(wt, w_in.h[l, :, C_MV:C_MV + 512], w_in, 8, 512)
                    v_tm(wt, hT, 8, vE["mvE"])
                    wt = wr.next()
                    load_w(wt, w_in.h[l, :, C_DC:C_DC + 256], w_in, 8, 256)
                    for tb in range(T // 512):
                        ps_c = [gemm_fm(wt, c * 128, 128, hT, tb * 512, 8) for c in range(2)]
                        cfs, sqs = [], []
                        for c in range(2):
                            cf, sq = qfr.next(), sqr.next()
                            k.act(cf[:], ps_c[c][:], AF.Copy)
                            k.act(sq[:], ps_c[c][:], AF.Square)
                            cfs.append(cf)
                            sqs.append(sq)
                        p2 = k.ps()
                        k.mm(p2[:], onesb[:], sqs[0][:], start=True, stop=False)
                        k.mm(p2[:], onesb[:], sqs[1][:], start=False, stop=True)
                        sd = sdr.next()
                        k.act(sd[:], p2[:], AF.Sqrt, bias=eps6[:])
                        k.recip(sd[:], sd[:])
                        for c in range(2):
                            k.stt("dve", cnT[:, c, tb * 512:(tb + 1) * 512], cfs[c][:], pvc(l, "kvn", c), sd[:],
                                  ALU.mult, ALU.mult)
                    wt = wr.next()
                    load_w(wt, kv_up.h[l, :, 0:512], kv_up, 2, 512)
                    qk_fm(wt, cnT, 2, 3, qkT["dkT"])
                    wt = wr.next()
                    load_w(wt, kv_up.h[l, :, 512:1024], kv_up, 2, 512)
                    v_tm(wt, cnT, 2, vE["dvE"])
                    wt = wr.next()
                    load_w(wt, w_in.h[l, :, C_DIQ:C_DIQ + 256], w_in, 8, 256)
                    for c in range(2):
                        for tb in range(T // 512):
                            p = gemm_fm(wt, c * 128, 128, hT, tb * 512, 8)
                            of = ofr.next()
                            k.copy("act", of[:], p[:])
                            dout(dv(qiT, qiT.h[c, :, tb * 512:(tb + 1) * 512]), of[:])
                    wt = wr.next()
                    for r4 in range(4):
                        load_any(wt[:, 0:8, r4 * 32:(r4 + 1) * 32],
                                 dv(w_in, w_in.h[l, :, C_DIK:C_DIK + 32].rearrange("(kc p) c -> p kc c", p=128)), 8, 32)
                    load_any(wt[:, 0:8, 128:136],
                             dv(w_in, w_in.h[l, :, C_DIW:C_DIW + 8].rearrange("(kc p) c -> p kc c", p=128)), 8, 8)
                    for tb in range(T // 512):
                        p = gemm_fm(wt, 0, 128, hT, tb * 512, 8)
                        of = ofr.next()
                        k.copy("act", of[:], p[:])
                        dout(dv(kiT, kiT.h[:, tb * 512:(tb + 1) * 512]), of[:])
                    for tt in range(T // 128):
                        p = k.ps()
                        for kc in range(8):
                            k.mm(p[:, 0:8], hT[:, kc, tt * 128:(tt + 1) * 128], wt[:, kc, 128:136],
                                 start=(kc == 0), stop=(kc == 7))
                        of = ofr.next()
                        k.copy("dve", of[:, 0:8], p[:, 0:8])
                        dout(dv(wiD, wiD.h[:, tt, :]), of[:, 0:8])
                    for g6 in range(6):
                        wt = wr.next()
                        load_w(wt, w_in.h[l, :, C_GATE + g6 * 512:C_GATE + (g6 + 1) * 512], w_in, 8, 512)
                        for c in range(4):
                            for tb in range(T // 512):
                                p = gemm_fm(wt, c * 128, 128, hT, tb * 512, 8)
                                ob = obr.next()
                                k.act(ob[:], p[:], AF.Sigmoid)
                                dout(dv(sgT, sgT.h[g6 * 4 + c, :, tb * 512:(tb + 1) * 512]), ob[:])
                    pfr = Rot(k, [128, S + 1], F32, "pf", 2, es=pes, init=0.0)
                    ddr = Rot(k, [128, S], F32, "dd", 1, es=pes)
                    mxr = Rot(k, [128, S], F32, "mx", 2, es=pes)
                    for role, (off, n) in RW_ROLES.items():
                        wt = wr.next()
                        load_w(wt, w_in.h[l, :, C_RW + off:C_RW + off + n], w_in, 8, n)
                        for c in range((n + 127) // 128):
                            m = min(128, n - c * 128)
                            for b in range(NB):
                                pf, dd, mx = pfr.next(), ddr.next(), mxr.next()
                                for tbl in range(4):
                                    p = gemm_fm(wt, c * 128, m, hT, b * S + tbl * 512, 8)
                                    k.copy("act", pf[0:m, 1 + tbl * 512:1 + (tbl + 1) * 512], p[0:m, :])
                                k.tt("dve", dd[0:m, :], pf[0:m, 0:S], pf[0:m, 1:S + 1], ALU.subtract)
                                k.stt("dve", mx[0:m, :], dd[0:m, :], pvc(l, "mu", role, c, m=m), pf[0:m, 1:S + 1],
                                      ALU.mult, ALU.add)
                                tsl = slice(b * S, (b + 1) * S)
                                if role in ("r", "k", "v"):
                                    dout(dv(rwT[role], rwT[role].h[c, :, tsl]), mx[:])
                                    if role == "v" and l == 0:
                                        dout(dv(vfirst, vfirst.h[c, :, tsl]), mx[:])
                                elif role == "wd":
                                    k.act(mx[0:64, :], mx[0:64, :], AF.Tanh)
                                    dout(dv(wdT, wdT.h[:, tsl]), mx[0:64, :])
                                elif role == "ad":
                                    dout(dv(adT, adT.h[:, tsl]), mx[0:64, :])
                                else:
                                    k.act(mx[0:m, :], mx[0:m, :], AF.Sigmoid)
                                    dout(dv(gsT, gsT.h[c, 0:m, tsl]), mx[0:m, :])


            def attention(kind, pes):
                hoff = 0 if kind == "m" else 8
                qd, kd, vd = (qkT["mqT"], qkT["mkT"], vE["mvE"]) if kind == "m" else (qkT["dqT"], qkT["dkT"], vE["dvE"])
                od = oT[kind]
                if True:
                    bt = k.tile([128, 8, 2, 128], F32, "bt", es=pes)
                    far = k.tile([128, 16], F32, "far", es=pes)
                    caus = k.tile([128, 128], F32, "caus", es=pes)
                    k.dma("sp", bt[:], dv(relb_d, relb_d.h[hoff:hoff + 8].rearrange("h t k q -> k h t q")), kind + "al0")
                    k.dma("sp", far[:], relfar_d[:], kind + "al1")
                    k.dma("sp", caus[:], caus_d[:], kind + "al2")
                    for h in range(8):
                        k.tt("dve", bt[:, h, 0, :], bt[:, h, 0, :], caus[:], ALU.add)
                    qT = k.tile([128, 4, S], BF16, "aqT", es=pes)
                    kT = k.tile([128, 4, S], BF16, "akT", es=pes)
                    vt = k.tile([128, 16, 520], BF16, "avt", es=pes)
                    Er = Rot(k, [128, 128], BF16, "E", 6, es=pes)
                    tmr = Rot(k, [128, 128], F32, "tm", 3, es=pes)
                    accr = Rot(k, [128, 65], F32, "acc", 6, es=pes)
                    rcr = Rot(k, [128, 1], F32, "rc", 6, es=pes)
                    otr = Rot(k, [128, 512], BF16, "otl", 2, es=pes)
                    oTr = Rot(k, [128, 4, 128], BF16, "oTt", 2, es=pes)
                    if kind == "m":
                        ks = k.tile([128, 4, 8], F32, "ks", es=pes)
                        kmh = k.tile([128, 4, 8], BF16, "kmh", es=pes)
                        kml = k.tile([128, 4, 8], BF16, "kml", es=pes)
                        kd_ = k.tile([128, 4, 8], F32, "kd_", es=pes)
                        pm = k.tile([128, 8, 64], F32, "pm", es=pes)
                        k.memset("dve", pm[:], 0.0)
                        for ni in range(8):
                            k.memset("dve", V(pm.h[:, ni, :].rearrange("p (h n) -> p h n", n=8)[:, :, ni:8], [pm.buf]), -1e30)
                        gr = Rot(k, [128, 64], F32, "gat", 2, es=pes)
                        cmr = Rot(k, [128, 512], F32, "cmp", 1, es=pes)
                        cnr = Rot(k, [128, 64], F32, "cnt", 2, es=pes)
                        mkr = Rot(k, [128, 64], F32, "msk", 2, es=pes)
                    else:
                        qi = k.tile([128, 3, S], F32, "qi", es=pes)
                        ki = k.tile([128, S], F32, "ki", es=pes)
                        wi = k.tile([128, 16, 8], F32, "wi", es=pes)
                        wa = k.tile([128, 16, 8], F32, "wa", es=pes)
                        wsg = k.tile([128, 16, 8], F32, "wsg", es=pes)
                        cq = k.tile([128, 128], F32, "cq", es=pes)
                        k.dma("sp", cq[:], causq_d[:], kind + "al3")
                        Isc = k.tile([128, S], F32, "Isc", es=pes)
                        wk = k.tile([128, S], F32, "wk", es=pes)
                        rrr = Rot(k, [128, 512], F32, "rr", 2, es=pes)
                        m8r = Rot(k, [128, 8], F32, "m8", 2, es=pes)
                        mskb = k.tile([128, S], BF16, "mskb", es=pes)
                        mTr = Rot(k, [128, 16, 128], BF16, "mT", 1, es=pes)
                        Emr = Rot(k, [128, 128], BF16, "Em", 6, es=pes)
                    for b in range(lim.get("nb", NB)):
                        tsl = slice(b * S, (b + 1) * S)
                        k.dma("sp", qT[:], dv(qd, qd.h[:, :, tsl].rearrange("c p t -> p c t")), kind + "al0")
                        k.dma("sp", kT[:], dv(kd, kd.h[:, :, tsl].rearrange("c p t -> p c t")), kind + "al1")
                        k.dma("sp", vt[:], dv(vd, vd.h[tsl, :].rearrange("(j p) f -> p j f", p=128)), kind + "al2")
                        if kind == "m":
                            k.reduce(ks[:], V(kT.h[:].rearrange("p c (n s) -> p c n s", s=256), [kT.buf]), ALU.add)
                            k.ts("dve", kd_[:], ks[:], 1.0 / 256, ALU.mult)
                            k.copy("dve", kmh[:], kd_[:])
                            k.tt("dve", kd_[:], kd_[:], kmh[:], ALU.subtract)
                            k.copy("dve", kml[:], kd_[:])
                        else:
                            k.dma("sp", [qi[32 * (h_ % 3):32 * (h_ % 3) + 32, h_ // 3, :] for h_ in range(8)],
                                  [dv(qiT, qiT.h[h_ // 4, 32 * (h_ % 4):32 * (h_ % 4) + 32, tsl]) for h_ in range(8)], kind + "al3")
                            k.dma("sp", ki[:], kiT[:, tsl], kind + "al4")
                            k.dma("sp", wi[:], dv(wiD, wiD.h[:, b * 16:(b + 1) * 16, :]), kind + "al5")
                            k.act(wa[:], wi[:], AF.Abs)
                            k.act(wsg[:], wi[:], AF.Sign)
                        for i in range(lim.get("qt", 16)):
                            qs = slice(i * 128, (i + 1) * 128)
                            ni = i // 2
                            if kind == "m":
                                g, cm, cn, msk = gr.next(), cmr.next(), cnr.next(), mkr.next()
                                for par in range(2):
                                    pg = k.ps()
                                    first = True
                                    base = 64 * par
                                    for c in range(4):
                                        for km in (kmh, kml):
                                            k.mm(pg[:, c * 8:(c + 1) * 8], qT[base:base + 64, c, qs], km[base:base + 64, c, :],
                                                 start=first, stop=True, skip_group_check=True)
                                            first = False
                                    k.tt("dve", V(g.h[:, :].rearrange("p (c par n) -> p c par n", par=2, n=8)[:, :, par, :], [g.buf]),
                                         V(pg.h[:, 0:32].rearrange("p (c n) -> p c n", n=8), [pg.buf]),
                                         V(pm.h[:, ni, :].rearrange("p (c par n) -> p c par n", par=2, n=8)[:, :, par, :], [pm.buf]), ALU.add)
                                g3 = g.h[:, :].rearrange("p (h n) -> p h n", n=8)
                                k.tt("dve", V(cm.h[:, :].rearrange("p (h n m) -> p h n m", n=8, m=8), [cm.buf]),
                                     V(g3.unsqueeze(2).broadcast_to([128, 8, 8, 8]), [g.buf]),
                                     V(g3.unsqueeze(3).broadcast_to([128, 8, 8, 8]), [g.buf]), ALU.is_gt)
                                k.reduce(cn[:], V(cm.h[:, :].rearrange("p (hn m) -> p hn m", m=8), [cm.buf]), ALU.add)
                                k.ts("dve", msk[:], cn[:], 3.0, ALU.is_lt)
                            else:
                                n = 128 * (i + 1)
                                for kb in range((n + 511) // 512):
                                    wdt = min(512, n - kb * 512)
                                    for h in range(8):
                                        pb_, cq_ = 32 * (h % 3), h // 3
                                        p = k.ps()
                                        k.mm(p[:, 0:wdt], qi[pb_:pb_ + 32, cq_, qs], ki[pb_:pb_ + 32, kb * 512:kb * 512 + wdt])
                                        rr = rrr.next()
                                        k.act(rr[:, 0:wdt], p[:, 0:wdt], AF.Relu, scale=wa[:, i, h:h + 1])
                                        if h == 0:
                                            k.ts("dve", Isc[:, kb * 512:kb * 512 + wdt], rr[:, 0:wdt], wsg[:, i, 0:1], ALU.mult)
                                        else:
                                            k.stt("dve", Isc[:, kb * 512:kb * 512 + wdt], rr[:, 0:wdt], wsg[:, i, h:h + 1],
                                                  Isc[:, kb * 512:kb * 512 + wdt], ALU.mult, ALU.add)
                                k.tt("dve", Isc[:, qs], Isc[:, qs], cq[:], ALU.add)
                                if i >= 2:
                                    k.copy("act", wk[:, 0:n], Isc[:, 0:n])
                                    for rd in range(32):
                                        m8 = m8r.next()
                                        k.max8(m8[:], wk[:, 0:n])
                                        if rd < 31:
                                            k.match_replace(wk[:, 0:n], m8[:], wk[:, 0:n], -1e30)
                                    k.ts("dve", mskb[:, 0:n], Isc[:, 0:n], m8[:, 7:8], ALU.is_ge)
                                else:
                                    k.ts("dve", mskb[:, 0:n], Isc[:, 0:n], -1e29, ALU.is_gt)
                                mT = mTr.next()
                                for j0 in range(0, i + 1, 8):
                                    p = k.ps()
                                    pb = pbf(p)
                                    nj = min(8, i + 1 - j0)
                                    for jj in range(nj):
                                        k.tr(V(pb.ap[:, jj * 128:(jj + 1) * 128], pb.bufs), mskb[:, (j0 + jj) * 128:(j0 + jj + 1) * 128], idb[:])
                                    k.copy("act", mT[:, j0:j0 + nj, :],
                                           V(pb.ap[:, 0:nj * 128].rearrange("p (j t) -> p j t", t=128), pb.bufs))
                            ot = otr.next()
                            for h in range(8):
                                c, base = h // 2, 64 * (h % 2)
                                fb = far[:, hoff + h:hoff + h + 1]

                                def make_E(j):
                                    ps_ = k.ps()
                                    k.mm(ps_[:, 0:128], kT[base:base + 64, c, j * 128:(j + 1) * 128], qT[base:base + 64, c, qs])
                                    E = Er.next()
                                    d_ = i - j
                                    if d_ >= 2:
                                        k.act(E[:], ps_[:, 0:128], AF.Exp, bias=fb, scale=0.125)
                                    else:
                                        tm = tmr.next()
                                        k.stt("dve", tm[:], ps_[:, 0:128], 0.125, bt[:, h, d_, :], ALU.mult, ALU.add)
                                        k.act(E[:], tm[:], AF.Exp)
                                    return E
                                acc = accr.next()
                                if kind == "m":
                                    for nblk in range(ni + 1):
                                        js = [j for j in (2 * nblk, 2 * nblk + 1) if j <= i]
                                        R = psR()
                                        for jx, j in enumerate(js):
                                            E = make_E(j)
                                            k.mm(R[:, 0:65], E[:], vt[:, j, h * 65:(h + 1) * 65], start=(jx == 0), stop=(jx == len(js) - 1))
                                        mcol = msk[:, h * 8 + nblk:h * 8 + nblk + 1]
                                        if nblk == 0:
                                            if nblk < ni:
                                                k.ts("dve", acc[:], R[:, 0:65], mcol, ALU.mult)
                                            else:
                                                k.copy("dve", acc[:], R[:, 0:65])
                                        elif nblk < ni:
                                            k.stt("dve", acc[:], R[:, 0:65], mcol, acc[:], ALU.mult, ALU.add)
                                        else:
                                            k.tt("dve", acc[:], R[:, 0:65], acc[:], ALU.add)
                                else:
                                    R = psR()
                                    for j in range(i + 1):
                                        E = make_E(j)
                                        Em = Emr.next()
                                        k.tt("dve", Em[:], E[:], mT[:, j, :], ALU.mult)
                                        k.mm(R[:, 0:65], Em[:], vt[:, j, h * 65:(h + 1) * 65], start=(j == 0), stop=(j == i))
                                    k.copy("dve", acc[:], R[:, 0:65])
                                rc = rcr.next()
                                k.recip(rc[:], acc[:, 64:65])
                                k.ts("dve", ot[:, h * 64:(h + 1) * 64], acc[:, 0:64], rc[:], ALU.mult)
                                yield
                            p = k.ps()
                            pb = pbf(p)
                            for c4 in range(4):
                                k.tr(V(pb.ap[:, c4 * 128:(c4 + 1) * 128], pb.bufs), ot[:, c4 * 128:(c4 + 1) * 128], idb[:])
                            oTt = oTr.next()
                            k.copy("act", oTt[:], V(pb.ap[:, 0:512].rearrange("p (c t) -> p c t", c=4), pb.bufs))
                            k.dma("sp", dv(od, od.h[:, :, b * S + i * 128:b * S + (i + 1) * 128].rearrange("c p t -> p c t")), oTt[:], kind + f"ao{i % 2}")

            kinds = [kd_ for kd_, ph_ in (("m", "A"), ("d", "X")) if ph_ in phases]
            for kd_ in kinds:
                k.s.barrier()
                with ExitStack() as pes_ax:
                    for _ in attention(kd_, pes_ax):
                        pass


            if "R" in phases:
                C0 = float(np.exp(-0.5))
                BT = 256
                NCH = BT // 64
                k.s.barrier()
                with ExitStack() as pes:
                    um = k.tile([64, 3, 64], F32, "um", es=pes)
                    k.dma("sp", um[:], umask_d[:], "rl0")
                    w2s = k.tile([128, 512], F32, "w2s", es=pes)
                    a2s = k.tile([128, 512], F32, "a2s", es=pes)
                    g2s = k.tile([128, 2, 512], BF16, "g2s", es=pes)
                    k.memset("dve", w2s[:], 0.0)
                    k.memset("dve", a2s[:], 0.0)
                    k.dma("sp", w2s[0:64, :], rw_w2[l, :, :], "rl1")
                    k.dma("sp", a2s[0:64, :], rw_a2[l, :, :], "rl2")
                    k.dma("pool", g2s[:, 0, :], rw_g2[l, 0:128, :], "pg0")
                    k.dma("pool", g2s[0:32, 1, :], rw_g2[l, 128:160, :], "pg1")
                    lnw = k.tile([64, 512], F32, "lnw", es=pes)
                    lnb = k.tile([64, 512], F32, "lnb", es=pes)
                    k.dma("sp", lnw[:], dv(rw_lnw, rw_lnw.h[l:l + 1, :].partition_broadcast(64)), "rl5")
                    k.dma("sp", lnb[:], dv(rw_lnb, rw_lnb.h[l:l + 1, :].partition_broadcast(64)), "rl6")
                    if l == 1:
                        vas = k.tile([128, 4, 128], F32, "vas", es=pes)
                        k.memset("dve", vas[:], 0.0)
                        vbs = k.tile([32, 512], F32, "vbs", es=pes)
                        k.dma("sp", vas[:, :, 0:32], dv(rw_va, rw_va.h[0].rearrange("(c p) r -> p c r", p=128)), "rl7")
                        k.dma("sp", vbs[:], rw_vb[0, :, :], "rl8")
                    ind2 = k.tile([128, 2], BF16, "ind2", es=pes)
                    k.memset("dve", ind2[:], 0.0)
                    k.memset("dve", ind2[0:64, 0:1], 1.0)
                    k.memset("dve", ind2[64:128, 1:2], 1.0)
                    rmask = k.tile([128, BT], F32, "rmask", es=pes)
                    k.memset("dve", rmask[:], 1.0)
                    k.memset("dve", V(rmask.h[:, :].rearrange("p (c t) -> p c t", t=64)[:, :, 0:1], [rmask.buf]), 0.0)
                    omka = k.tile([128, 4], F32, "omka", es=pes)
                    for c in range(4):
                        k.ts("dve", omka[:, c:c + 1], pvc(l, "ka", c), -1.0, ALU.mult, 1.0, ALU.add)
                    epsg = k.tile([128, 1], F32, "epsg", es=pes)
                    k.memset("dve", epsg[:], 64e-5)
                    epsk = k.tile([128, 1], F32, "epsk", es=pes)
                    k.memset("dve", epsk[:], 1e-24)

                    def ft(name, n=1):
                        return Rot(k, [128, 4, BT], F32, name, n, es=pes)
                    rTr, kTr_, vTr_, vfr = ft("rT"), ft("kTl"), ft("vTl"), ft("vfl", 1)
                    wdr = Rot(k, [128, BT], F32, "wdl", 1, es=pes, init=0.0)
                    adr = Rot(k, [128, BT], F32, "adl", 1, es=pes, init=0.0)
                    gsr = Rot(k, [128, 2, BT], BF16, "gsl", 1, es=pes)
                    sgw, aa, Ls, G_, Gi, Gp = ft("sgw", 1), ft("aa", 1), ft("Ls", 1), ft("G_", 1), ft("Gi", 1), ft("Gp", 1)
                    kkn, t1f, t2f = ft("kkn", 1), ft("t1f", 1), ft("t2f", 1)
                    ARer = Rot(k, [128, 4, NCH, 2, 64], BF16, "ARe", 1, es=pes, init=0.0)
                    ARor = Rot(k, [128, 4, NCH, 2, 64], BF16, "ARo", 1, es=pes, init=0.0)
                    def fb_(name):
                        return Rot(k, [128, 4, BT], BF16, name, 1, es=pes)
                    bhbr, khbr, btbr, ktbr, vbr, prbr = fb_("bhb"), fb_("khb"), fb_("btb"), fb_("ktb"), fb_("vbb"), fb_("prb")
                    Hbr = Rot(k, [128, 4, 64], BF16, "Hb", 2, es=pes)

                    GCr = Rot(k, [128, 4, NCH], F32, "GC", 2, es=pes)
                    tokr = {nm: Rot(k, [64, 512], BF16, "tk" + nm, 3, es=pes) for nm in ("b", "k", "v")}
                    m13r = Rot(k, [64, 8, 2, 64], BF16, "m13", 3, es=pes)
                    m24r = Rot(k, [64, 8, 2, 64], BF16, "m24", 3, es=pes)
                    sq_ = {nm: Rot(k, [64, 8, 64], BF16, "q" + nm, (14 if nm == "P" else 3), es=pes) for nm in ("M", "N", "P", "Q")}
                    xsr = Rot(k, [64, 512], BF16, "xs", 2, es=pes)
                    usr = Rot(k, [64, 512], BF16, "us", 2, es=pes)
                    osr = Rot(k, [64, 512], F32, "os", 2, es=pes)
                    Hr = Rot(k, [128, 4, 64], F32, "H", 2, es=pes)
                    s8r = Rot(k, [64, 8], F32, "s8", 6, es=pes)
                    cer = Rot(k, [64, 512], F32, "ce", 2, es=pes)
                    sqr2 = Rot(k, [64, 512], F32, "sq2", 2, es=pes)
                    ybr = Rot(k, [64, 512], BF16, "yb", 2, es=pes)
                    oTr2 = Rot(k, [128, 4, BT], BF16, "oTr", 2, es=pes)
                    t1s = k.tile([32, BT], F32, "t1s", es=pes)
                    evi = [0]

                    def evac(dst, src):
                        evi[0] += 1
                        k.copy("act" if evi[0] % 2 else "dve", dst, src)

                    def flat(t):
                        return V(t.h[:].rearrange("p c t -> p (c t)"), [t.buf])

                    def hv(t, h):
                        return t[:, h, :]

                    for b in range(lim.get("nb", NB)):
                        H = Hr.next()
                        k.memset("dve", H[:], 0.0)
                        Hb = Hbr.next()
                        k.memset("dve", Hb[:], 0.0)
                        for tb in range(lim.get("rb", S // BT)):
                            t0 = b * S + tb * BT
                            tsl = slice(t0, t0 + BT)
                            rT, kTl, vTl = rTr.next(), kTr_.next(), vTr_.next()
                            wdl, adl, gsl = wdr.next(), adr.next(), gsr.next()
                            k.dma("sp", rT[:], dv(rwT["r"], rwT["r"].h[:, :, tsl].rearrange("c p t -> p c t")), "rl0")
                            k.dma("sp", kTl[:], dv(rwT["k"], rwT["k"].h[:, :, tsl].rearrange("c p t -> p c t")), "rl1")
                            k.dma("sp", vTl[:], dv(rwT["v"], rwT["v"].h[:, :, tsl].rearrange("c p t -> p c t")), "rl2")
                            k.dma("sp", wdl[0:64, :], wdT[:, tsl], "rl3")
                            k.dma("sp", adl[0:64, :], adT[:, tsl], "rl4")
                            k.dma("pool", gsl[:], dv(gsT, gsT.h[:, :, tsl].rearrange("c p t -> p c t")), "pg2")
                            sg_, a_, L_, G, GI, GP = sgw.next(), aa.next(), Ls.next(), G_.next(), Gi.next(), Gp.next()
                            kn, t1, t2 = kkn.next(), t1f.next(), t2f.next()
                            for c in range(4):
                                cs = slice(c * 128, (c + 1) * 128)
                                p = k.ps()
                                k.mm(p[:, 0:BT], w2s[:, cs], wdl[:, :])
                                k.act(sg_[:, c, :], p[:, 0:BT], AF.Sigmoid, bias=pvc(l, "w0", c))
                                p = k.ps()
                                k.mm(p[:, 0:BT], a2s[:, cs], adl[:, :])
                                k.act(a_[:, c, :], p[:, 0:BT], AF.Sigmoid, bias=pvc(l, "a0", c))
                            if l == 1:
                                vf = vfr.next()
                                k.dma("sp", vf[:], dv(vfirst, vfirst.h[:, :, tsl].rearrange("c p t -> p c t")), "rl6")
                                p = k.ps()
                                for c in range(4):
                                    k.mm(p[:, 0:BT], vas[:, c, :], vTl[:, c, :], start=(c == 0), stop=(c == 3))
                                k.copy("act", t1s[:], p[0:32, 0:BT])
                                for c in range(4):
                                    p = k.ps()
                                    k.mm(p[:, 0:BT], vbs[0:32, c * 128:(c + 1) * 128], t1s[0:32, :])
                                    k.act(t1[:, c, :], p[:, 0:BT], AF.Sigmoid, bias=pvc(l, "v0", c))
                                k.tt("dve", flat(vf), flat(vf), flat(vTl), ALU.subtract)
                                k.tt("dve", flat(vf), flat(vf), flat(t1), ALU.mult)
                                k.tt("dve", flat(vTl), flat(vTl), flat(vf), ALU.add)
                            for c in range(4):
                                k.ts("dve", kn[:, c, :], kTl[:, c, :], pvc(l, "kk", c), ALU.mult)
                            k.tt("dve", flat(t1), flat(kn), flat(kn), ALU.mult)
                            for c in range(4):
                                p = k.ps()
                                k.mm(p[:, 0:BT], blkf[:], t1[:, c, :])
                                k.act(t2[:, c, :], p[:, 0:BT], AF.Sqrt, bias=epsk[:], scale=64.0)
                            k.recip(flat(t2), flat(t2))
                            k.tt("dve", flat(kn), flat(kn), flat(t2), ALU.mult)
                            for c in range(4):
                                k.ts("dve", t1[:, c, :], a_[:, c, :], pvc(l, "ka", c), ALU.mult, omka[:, c:c + 1], ALU.add)
                            k.tt("dve", flat(kTl), flat(kTl), flat(t1), ALU.mult)
                            for c in range(4):
                                k.scan(L_[:, c, :], rmask[:], sg_[:, c, :], 0.0, ALU.mult, ALU.add)
                            k.act(flat(G), flat(L_), AF.Exp, scale=-C0)
                            k.act(flat(GI), flat(L_), AF.Exp, scale=C0)
                            k.tt("dve", flat(t2), flat(L_), flat(sg_), ALU.subtract)
                            k.act(flat(GP), flat(t2), AF.Exp, scale=-C0)
                            GC = GCr.next()
                            k.copy("dve", GC[:], V(G.h[:].rearrange("p c (ch t) -> p c ch t", t=64)[:, :, :, 63], [G.buf]))
                            ARx = (ARer.next(), ARor.next())
                            bh, kh, bt2, kt2 = t1, t2, sg_, a_

                            def v4(t):
                                return V(t.h[:].rearrange("p c (ch t) -> p c ch t", t=64), [t.buf])
                            def v4h(t, hb):
                                return V(t.h[hb * 64:(hb + 1) * 64].rearrange("p c (ch t) -> p c ch t", t=64), [t.buf])
                            for hb in range(2):
                                AR_ = ARx[hb]
                                k.stt("dve", V(AR_.h[hb * 64:(hb + 1) * 64, :, :, 0, :], [AR_.buf]), v4h(kn, hb), -1.0, v4h(GP, hb), ALU.mult, ALU.mult)
                                k.tt("dve", V(AR_.h[hb * 64:(hb + 1) * 64, :, :, 1, :], [AR_.buf]), v4h(rT, hb), v4h(G, hb), ALU.mult)
                            prd = rT
                            k.tt("dve", flat(prd), flat(rT), flat(kTl), ALU.mult)
                            for c in range(4):
                                k.ts("dve", prd[:, c, :], prd[:, c, :], pvc(l, "rk", c), ALU.mult)
                            k.tt("dve", flat(bh), flat(kn), flat(a_), ALU.mult)
                            k.tt("dve", flat(bh), flat(bh), flat(GI), ALU.mult)
                            k.tt("dve", flat(kh), flat(kTl), flat(GI), ALU.mult)
                            gcb = V(GC.h[:].unsqueeze(3).broadcast_to([128, 4, NCH, 64]), [GC.buf])
                            bhb, khb, btb, ktb, vbb, prb = bhbr.next(), khbr.next(), btbr.next(), ktbr.next(), vbr.next(), prbr.next()
                            k.tt("dve", v4(btb), v4(bh), gcb, ALU.mult)
                            k.tt("dve", v4(ktb), v4(kh), gcb, ALU.mult)
                            k.copy("act", flat(bhb), flat(bh))
                            k.copy("act", flat(khb), flat(kh))
                            k.copy("act", flat(vbb), flat(vTl))
                            k.copy("act", flat(prb), flat(prd))
                            oTt = oTr2.next()
                            def prep_chunk(ch):
                                csl = slice(ch * 64, (ch + 1) * 64)
                                tok = {}
                                for nm, src in (("b", btb), ("k", ktb), ("v", vbb)):
                                    p = k.ps()
                                    pbb = pbf(p)
                                    for c in range(4):
                                        k.tr(V(pbb.ap[0:64, c * 128:(c + 1) * 128], pbb.bufs), src[:, c, csl], idb[:])
                                    tk = tokr[nm].next()
                                    evac(tk[:], V(pbb.ap[0:64, 0:512], pbb.bufs))
                                    tok[nm] = tk
                                m13, m24 = m13r.next(), m24r.next()
                                for (dst, lh) in ((m13, bhb), (m24, khb)):
                                    for half in range(2):
                                        p = k.ps()
                                        for hh in range(4):
                                            h = half * 4 + hh
                                            c, base = h // 2, 64 * (h % 2)
                                            AR = ARx[h % 2]
                                            k.mm(p[0:64, hh * 128:(hh + 1) * 128], lh[:, c, csl],
                                                 V(AR.h[:, c, ch, :, :].rearrange("p a t -> p (a t)"), [AR.buf]),
                                                 start=(hh == 0), stop=True, skip_group_check=True)
                                        k.tt("dve", dst[:, half * 4:(half + 1) * 4, :, :],
                                             V(p.h[0:64, :].rearrange("p (h a t) -> p h a t", h=4, a=2), [p.buf]),
                                             V(um.h[:, 0:2, :].unsqueeze(1).broadcast_to([64, 4, 2, 64]), [um.buf]), ALU.mult)
                                Mx, Nx, Px, Qx = (sq_[n_].next() for n_ in ("M", "N", "P", "Q"))
                                p = k.ps()
                                for h in range(8):
                                    c, base = h // 2, 64 * (h % 2)
                                    k.mm(p[0:64, h * 64:(h + 1) * 64], ARx[h % 2][:, c, ch, 0, :], bhb[:, c, csl],
                                         start=(h == 0), stop=True, skip_group_check=True)
                                k.tt("dve", Nx[:], V(p.h[0:64, :].rearrange("p (h t) -> p h t", h=8), [p.buf]),
                                     V(um.h[:, 2:3, :].broadcast_to([64, 8, 64]), [um.buf]), ALU.mult)
                                k.copy("act", Mx[:], m13[:, :, 0, :])
                                idb8 = V(idf.h[0:64, 0:64].unsqueeze(1).broadcast_to([64, 8, 64]), [idf.buf])
                                k.tt("dve", Px[:], Mx[:], idb8, ALU.add)
                                k.tt("dve", Qx[:], Nx[:], idb8, ALU.add)
                                for lvl in range(1, 6):
                                    last = lvl == 5
                                    M2_, N2_, P2_, Q2_ = (sq_[n_].next() for n_ in ("M", "N", "P", "Q"))
                                    p = k.ps()
                                    for h in range(8):
                                        k.mm(p[0:64, h * 64:(h + 1) * 64], hv(Nx, h), hv(Mx, h), start=(h == 0), stop=True, skip_group_check=True)
                                    evac(M2_[:], V(p.h[0:64, :].rearrange("p (h t) -> p h t", h=8), [p.buf]))
                                    if not last:
                                        p = k.ps()
                                        for h in range(8):
                                            k.mm(p[0:64, h * 64:(h + 1) * 64], hv(Mx, h), hv(Nx, h), start=(h == 0), stop=True, skip_group_check=True)
                                        evac(N2_[:], V(p.h[0:64, :].rearrange("p (h t) -> p h t", h=8), [p.buf]))
                                    p = k.ps()
                                    for h in range(8):
                                        k.mm(p[0:64, h * 64:(h + 1) * 64], hv(Qx, h), hv(M2_, h), start=(h == 0), stop=True, skip_group_check=True)
                                    k.tt("dve", P2_[:], V(p.h[0:64, :].rearrange("p (h t) -> p h t", h=8), [p.buf]), Px[:], ALU.add)
                                    if not last:
                                        p = k.ps()
                                        for h in range(8):
                                            k.mm(p[0:64, h * 64:(h + 1) * 64], hv(Px, h), hv(N2_, h), start=(h == 0), stop=True, skip_group_check=True)
                                        k.tt("dve", Q2_[:], V(p.h[0:64, :].rearrange("p (h t) -> p h t", h=8), [p.buf]), Qx[:], ALU.add)
                                    Mx, Nx, Px, Qx = M2_, N2_, P2_, Q2_
                                return tok, m13, m24, Px

                            prepared = {0: prep_chunk(0)}
                            for ch in range(NCH):
                                csl = slice(ch * 64, (ch + 1) * 64)
                                if ch + 1 < NCH:
                                    prepared[ch + 1] = prep_chunk(ch + 1)
                                tok, m13, m24, Px = prepared.pop(ch)
                                px = psR()
                                for h in range(8):
                                    c, base = h // 2, 64 * (h % 2)
                                    k.mm(px[0:64, h * 64:(h + 1) * 64], ARx[h % 2][:, c, ch, 0, :], Hb[:, c, :],
                                         start=(h == 0), stop=False, skip_group_check=True)
                                for h in range(8):
                                    k.mm(px[0:64, h * 64:(h + 1) * 64], m24[:, h, 0, :], tok["v"][:, h * 64:(h + 1) * 64],
                                         start=False, stop=True, skip_group_check=True)
                                xs = xsr.next()
                                k.copy("act", xs[:], px[0:64, :])
                                pu = psR()
                                for h in range(8):
                                    k.mm(pu[0:64, h * 64:(h + 1) * 64], hv(Px, h), xs[:, h * 64:(h + 1) * 64],
                                         start=(h == 0), stop=True, skip_group_check=True)
                                us = usr.next()
                                k.copy("act", us[:], pu[0:64, :])
                                po = k.ps()
                                for h in range(8):
                                    c, base = h // 2, 64 * (h % 2)
                                    k.mm(po[0:64, h * 64:(h + 1) * 64], ARx[h % 2][:, c, ch, 1, :], Hb[:, c, :],
                                         start=(h == 0), stop=False, skip_group_check=True)
                                for h in range(8):
                                    k.mm(po[0:64, h * 64:(h + 1) * 64], m13[:, h, 1, :], us[:, h * 64:(h + 1) * 64],
                                         start=False, stop=False, skip_group_check=True)
                                for h in range(8):
                                    k.mm(po[0:64, h * 64:(h + 1) * 64], m24[:, h, 1, :], tok["v"][:, h * 64:(h + 1) * 64],
                                         start=False, stop=True, skip_group_check=True)
                                ph = psR()
                                for c in range(4):
                                    cs = slice(c * 128, (c + 1) * 128)
                                    k.mm(ph[:, cs], tok["b"][:, cs], us[:, cs], start=(c == 0), stop=False, skip_group_check=True)
                                for c in range(4):
                                    cs = slice(c * 128, (c + 1) * 128)
                                    k.mm(ph[:, cs], tok["k"][:, cs], tok["v"][:, cs], start=False, stop=True, skip_group_check=True)
                                Hn = Hr.next()
                                for c in range(4):
                                    for hb in range(2):
                                        ps_ = slice(hb * 64, (hb + 1) * 64)
                                        k.stt("dve", Hn[ps_, c, :], H[ps_, c, :], GC[ps_, c, ch:ch + 1],
                                              ph[ps_, c * 128 + hb * 64:c * 128 + (hb + 1) * 64], ALU.mult, ALU.add)
                                H = Hn
                                Hb = Hbr.next()
                                k.copy("act", Hb[:], Hn[:])
                                osb = osr.next()
                                k.copy("act", osb[:], po[0:64, :])
                                o3 = V(osb.h[:, :].rearrange("p (h v) -> p h v", h=8), [osb.buf])
                                s1, s2, bon = s8r.next(), s8r.next(), s8r.next()
                                k.reduce(s1[:], o3, ALU.add)
                                k.ts("dve", s1[:], s1[:], 1.0 / 64, ALU.mult)
                                ce, sq2 = cer.next(), sqr2.next()
                                ce3 = V(ce.h[:, :].rearrange("p (h v) -> p h v", h=8), [ce.buf])
                                k.tt("dve", ce3, o3, V(s1.h[:, :].unsqueeze(2).broadcast_to([64, 8, 64]), [s1.buf]), ALU.subtract)
                                k.tt("dve", sq2[:], ce[:], ce[:], ALU.mult)
                                k.reduce(s2[:], V(sq2.h[:, :].rearrange("p (h v) -> p h v", h=8), [sq2.buf]), ALU.add)
                                k.act(s2[:], s2[:], AF.Sqrt, bias=epsg[0:64, :], scale=1.0 / 64)
                                k.recip(s2[:], s2[:])
                                k.tt("dve", ce3, ce3, V(s2.h[:, :].unsqueeze(2).broadcast_to([64, 8, 64]), [s2.buf]), ALU.mult)
                                k.tt("dve", ce[:], ce[:], lnw[:], ALU.mult)
                                k.tt("dve", ce[:], ce[:], lnb[:], ALU.add)
                                pb_ = k.ps()
                                for c in range(4):
                                    k.mm(pb_[0:64, 2 * c:2 * c + 2], prb[:, c, csl], ind2[:], start=(c == 0), stop=True, skip_group_check=True)
                                k.copy("act", bon[:], pb_[0:64, 0:8])
                                k.tt("dve", V(sq2.h[:, :].rearrange("p (h v) -> p h v", h=8), [sq2.buf]),
                                     V(tok["v"].h[:, :].rearrange("p (h v) -> p h v", h=8), [tok["v"].buf]),
                                     V(bon.h[:, :].unsqueeze(2).broadcast_to([64, 8, 64]), [bon.buf]), ALU.mult)
                                k.tt("dve", ce[:], ce[:], sq2[:], ALU.add)
                                pg_ = k.ps()
                                k.mm(pg_[0:64, :], gsl[:, 0, csl], g2s[:, 0, :], start=True, stop=False)
                                k.mm(pg_[0:64, :], gsl[0:32, 1, csl], g2s[0:32, 1, :], start=False, stop=True)
                                yb = ybr.next()
                                k.tt("dve", yb[:], pg_[0:64, :], ce[:], ALU.mult)
                                pt = k.ps()
                                ptb = pbf(pt)
                                for c in range(4):
                                    k.tr(V(ptb.ap[:, c * 64:(c + 1) * 64], ptb.bufs), yb[:, c * 128:(c + 1) * 128], idb[0:64, 0:64])
                                k.copy("act", oTt[:, :, csl], V(ptb.ap[:, 0:256].rearrange("p (c t) -> p c t", c=4), ptb.bufs))
                            k.dma("sp", dv(oT["r"], oT["r"].h[:, :, tsl].rearrange("c p t -> p c t")), oTt[:], f"ro{tb % 2}")

            if "M" in phases:
                k.s.barrier()
                with ExitStack() as pes:
                    wb = k.tile([128, 12, D], BF16, "wb", es=pes)
                    wo = k.tile([128, 8, D], BF16, "wo", es=pes)
                    stage[0] = Rot(k, [128, 8, 512], F32, "stgm", 1, es=pes)
                    for i in range(3):
                        for hf in range(2):
                            load_any(wb[:, i * 4:(i + 1) * 4, hf * 512:(hf + 1) * 512],
                                     dv(w_branch, w_branch.h[l, i, :, hf * 512:(hf + 1) * 512].rearrange("(kc p) c -> p kc c", p=128)), 4, 512)
                    for hf in range(2):
                        load_any(wo[:, :, hf * 512:(hf + 1) * 512],
                                 dv(w_o, w_o.h[l, :, hf * 512:(hf + 1) * 512].rearrange("(kc p) c -> p kc c", p=128)), 8, 512)
                    otr = Rot(k, [128, 12, 512], BF16, "ot", 2, es=pes)
                    sgr = Rot(k, [128, 24, 512], BF16, "sgl", 2, es=pes)
                    zTr = Rot(k, [128, 8, 512], BF16, "zT", 2, es=pes)
                    t0r = Rot(k, [128, 512], F32, "t0_", 3, es=pes)
                    xr = Rot(k, [128, D], F32, "xrm", 3, es=pes)
                    for tb in range(lim.get("mtb", T // 512)):
                        ot, sg, zT = otr.next(), sgr.next(), zTr.next()
                        tsl = slice(tb * 512, (tb + 1) * 512)
                        for i, nm in enumerate(("m", "r", "d")):
                            k.dma("sp", ot[:, i * 4:(i + 1) * 4, :], dv(oT[nm], oT[nm].h[:, :, tsl].rearrange("c p t -> p c t")),
                                  f"ml{i}")
                        k.dma("sp", sg[:], dv(sgT, sgT.h[:, :, tsl].rearrange("c p t -> p c t")), "ml3")
                        for cc in range(8):
                            acc = None
                            for i in range(3):
                                p = k.ps()
                                for kc in range(4):
                                    k.mm(p[:], wb[:, i * 4 + kc, cc * 128:(cc + 1) * 128], ot[:, i * 4 + kc, :],
                                         start=(kc == 0), stop=(kc == 3))
                                t0 = t0r.next()
                                k.tt("dve", t0[:], p[:], sg[:, i * 8 + cc, :], ALU.mult)
                                if acc is None:
                                    acc = t0
                                elif i == 1:
                                    k.tt("dve", t0[:], t0[:], acc[:], ALU.add)
                                    acc = t0
                                else:
                                    k.tt("dve", zT[:, cc, :], t0[:], acc[:], ALU.add)
                        for tt in range(4):
                            xt = xr.next()
                            r0 = tb * 512 + tt * 128
                            k.dma("sp", xt[:], xsrc[r0:r0 + 128, :], f"xl{tt % 2}")
                            for nb in range(2):
                                p = k.ps()
                                for kc in range(8):
                                    k.mm(p[:], zT[:, kc, tt * 128:(tt + 1) * 128], wo[:, kc, nb * 512:(nb + 1) * 512],
                                         start=(kc == 0), stop=(kc == 7))
                                k.tt("dve", xt[:, nb * 512:(nb + 1) * 512], p[:], xt[:, nb * 512:(nb + 1) * 512], ALU.add)
                            k.dma("sp", xm_d[r0:r0 + 128, :], xt[:], f"xs{tt % 2}")

            if "F" in phases:
                for b in range(lim.get("nb", NB)):
                    k.s.barrier()
                    with ExitStack() as pes:
                        hT = k.tile([128, 8, S], BF16, "hTf", es=pes)
                        aT = k.tile([128, 22, S], BF16, "aT", es=pes)
                        norm_transpose(pes, xm_d, (norm_ffn, norm_ffn.h[l:l + 1, :]), hT, b * S, S // 128, "f")
                        stage[0] = Rot(k, [128, 8, 512], F32, "stgf", 1, es=pes)
                        wfr = Rot(k, [128, 8, 512], BF16, "wf", 2, es=pes)
                        sgr = Rot(k, [128, 512], F32, "sgf", 2, es=pes)

                        def f_dma(cb):
                            stg = stage[0].next()
                            wq[0] += 1
                            k.dma("sp", [stg[:, :, 0:256], stg[:, :, 256:512]],
                                  [dv(w_ffn_in, w_ffn_in.h[l, :, cb * 256:(cb + 1) * 256].rearrange("(kc p) c -> p kc c", p=128)),
                                   dv(w_ffn_in, w_ffn_in.h[l, :, DFF + cb * 256:DFF + (cb + 1) * 256].rearrange("(kc p) c -> p kc c", p=128))],
                                  f"w{wq[0] % 2}")
                            return stg
                        stg_next = f_dma(0)
                        for cb in range(11):
                            wf = wfr.next()
                            k.copy("act" if cb % 2 else "dve", wf[:], stg_next[:])
                            if cb + 1 < 11:
                                stg_next = f_dma(cb + 1)
                            for c in range(2):
                                for tb in range(S // 512):
                                    pg = gemm_fm(wf, c * 128, 128, hT, tb * 512, 8)
                                    pu = gemm_fm(wf, 256 + c * 128, 128, hT, tb * 512, 8)
                                    sg = sgr.next()
                                    k.act(sg[:], pg[:], AF.Silu)
                                    k.tt("dve", aT[:, cb * 2 + c, tb * 512:(tb + 1) * 512], pu[:], sg[:], ALU.mult)
                        wout = k.tile([128, 22, 512], BF16, "wout", es=pes)
                        xr = Rot(k, [128, 512], F32, "xrf", 2, es=pes)
                        xts = {}
                        for nb in range(2):
                            for (c0_, c1_) in ((0, 8), (8, 16), (16, 22)):
                                load_any(wout[:, c0_:c1_, :],
                                         dv(w_ffn_out, w_ffn_out.h[l, c0_ * 128:c1_ * 128, nb * 512:(nb + 1) * 512].rearrange("(kc p) c -> p kc c", p=128)),
                                         c1_ - c0_, 512)
                            for tt in range(S // 128):
                                r0 = b * S + tt * 128
                                xt = xr.next()
                                k.dma("sp", xt[:, 0:512], xm_d[r0:r0 + 128, nb * 512:(nb + 1) * 512], f"xl{tt % 2}")
                                p = k.ps()
                                for c in range(22):
                                    k.mm(p[:], aT[:, c, tt * 128:(tt + 1) * 128], wout[:, c, :], start=(c == 0), stop=(c == 21))
                                k.tt("dve", xt[:, 0:512], p[:], xt[:, 0:512], ALU.add)
                                k.dma("sp", xdst[r0:r0 + 128, nb * 512:(nb + 1) * 512], xt[:, 0:512], f"xs{tt % 2}")
        st = k.emit()
    return nc, st


_CACHE = {}


def kernel(**inputs):
    inp = {k_: np.asarray(v) for k_, v in inputs.items()}
    if "nc" not in _CACHE:
        _CACHE["nc"] = build()[0]
    nc = _CACHE["nc"]
    consts = host_consts(inp)
    in_maps = []
    shared = {}
    for nm in ("w_in", "norm_mix", "norm_ffn", "dsa_kv_up", "w_branch", "w_o", "w_ffn_in", "w_ffn_out",
               "rwkv_w2", "rwkv_a2", "rwkv_g2", "rwkv_va", "rwkv_vb", "rwkv_ln_w", "rwkv_ln_b"):
        shared[nm] = np.ascontiguousarray(inp[nm], dtype=np.float32)
    shared.update(consts)
    for c in range(NCORE):
        m = dict(shared)
        m["x"] = np.ascontiguousarray(inp["x"][c * NB:(c + 1) * NB].reshape(T, D), dtype=np.float32)
        in_maps.append(m)
    res = run_bass_kernel_spmd(nc, in_maps, core_ids=list(range(NCORE)))
    out = np.concatenate([np.asarray(r["y"]).reshape(NB, S, D) for r in res.results], axis=0)
    return out.astype(np.float32)
```

```python
import numpy as np
from contextlib import ExitStack
import concourse.bass as bass
import concourse.mybir as mybir

F32 = mybir.dt.float32
BF16 = mybir.dt.bfloat16
AF = mybir.ActivationFunctionType
ALU = mybir.AluOpType
AX = mybir.AxisListType


class Buf:
    __slots__ = ("name", "wr", "rd", "nowaw")

    def __init__(self, name=""):
        self.name = name
        self.wr = {}
        self.rd = {}
        self.nowaw = False


class V:
    __slots__ = ("ap", "bufs")

    def __init__(self, ap, bufs):
        self.ap = ap
        self.bufs = bufs


class Tile:
    def __init__(self, handle, name):
        self.h = handle
        self.name = name
        self.buf = Buf(name)
        self.subs = {}

    def __getitem__(self, idx):
        return V(self.h[idx], [self.buf])

    def sub(self, key):
        b = self.subs.get(key)
        if b is None:
            b = self.subs[key] = Buf(f"{self.name}/{key}")
        return _Sub(self, b)

    def subs_of(self, keys):
        return [self.sub(k).b for k in keys]


class _Sub:
    def __init__(self, t, b):
        self.t = t
        self.b = b

    def __getitem__(self, idx):
        return V(self.t.h[idx], [self.b])


class Op:
    __slots__ = ("eng", "fn", "deps", "signal", "sem", "sigval", "chan", "n")


ENGS = ("pe", "act", "dve", "pool", "sp")


class Sched:
    def __init__(self, nc):
        self.nc = nc
        self.ops = []
        self.chan_last = {}
        self.E = {"pe": nc.tensor, "act": nc.scalar, "dve": nc.vector, "pool": nc.gpsimd, "sp": nc.sync}

    def add(self, eng, fn, reads, writes, chan=None, n=1):
        idx = len(self.ops)
        key = eng if chan is None else "d:" + chan
        deps = set()
        for b in reads:
            deps.update(b.wr.values())
        for b in writes:
            if not b.nowaw:
                deps.update(b.wr.values())
            deps.update(b.rd.values())
        for b in reads:
            b.rd[key] = idx
        for b in writes:
            if b.rd:
                b.wr = {key: idx}
                b.rd = {}
            else:
                b.wr[key] = idx
        if chan is not None:
            if chan in self.chan_last:
                deps.add(self.chan_last[chan])
            self.chan_last[chan] = idx
        deps.discard(idx)
        best = {}
        for j in deps:
            o = self.ops[j]
            k2 = o.eng if o.chan is None else "d:" + o.chan
            if k2 == "pe" and eng == "pe" and chan is None:
                continue
            if k2 not in best or best[k2] < j:
                best[k2] = j
        op = Op()
        op.eng = eng
        op.fn = fn
        op.deps = sorted(best.values())
        op.signal = chan is not None
        op.sem = None
        op.sigval = None
        op.chan = chan
        op.n = n
        for j in op.deps:
            self.ops[j].signal = True
        self.ops.append(op)
        return idx

    def barrier(self):
        last = {}
        for idx, o in enumerate(self.ops):
            if o.fn is not None:
                last[o.eng if o.chan is None else "d:" + o.chan] = idx
        deps = sorted(set(last.values()))
        for e in ENGS:
            op = Op()
            op.eng = e
            op.fn = None
            op.deps = list(deps)
            op.signal = False
            op.sem = None
            op.sigval = None
            op.chan = None
            op.n = 0
            self.ops.append(op)
        for j in deps:
            self.ops[j].signal = True

    def emit(self, es):
        nc = self.nc
        sems = {e: es.enter_context(nc.semaphore("s_" + e)) for e in ENGS}
        chans = sorted(set(o.chan for o in self.ops if o.chan is not None))
        for c in chans:
            sems["d:" + c] = es.enter_context(nc.semaphore("d_" + c))
        cnt = {k: 0 for k in sems}
        seen = {e: {} for e in ENGS}
        nwait = 0
        for op in self.ops:
            E = self.E[op.eng]
            sn = seen[op.eng]
            for j in op.deps:
                d = self.ops[j]
                if sn.get(d.sem, 0) < d.sigval:
                    E.wait_ge(sems[d.sem], d.sigval)
                    sn[d.sem] = d.sigval
                    nwait += 1
            if op.fn is None:
                continue
            ins = op.fn(E)
            if op.chan is not None:
                sk = "d:" + op.chan
                if not isinstance(ins, (list, tuple)):
                    ins = [ins]
                assert len(ins) == op.n
                for i_ in ins:
                    i_.then_inc(sems[sk], 16)
                cnt[sk] += 16 * op.n
                op.sem = sk
                op.sigval = cnt[sk]
            elif op.signal:
                ins.then_inc(sems[op.eng], 1)
                cnt[op.eng] += 1
                op.sem = op.eng
                op.sigval = cnt[op.eng]
        sp = self.E["sp"]
        for k, v in cnt.items():
            if v > 0 and seen["sp"].get(k, 0) < v:
                sp.wait_ge(sems[k], v)
        return dict(n_ops=len(self.ops), n_wait=nwait, cnt=cnt)


def _bufs(*vs):
    out = []
    for v in vs:
        if isinstance(v, V):
            out.extend(v.bufs)
    return out


def _ap(v):
    return v.ap if isinstance(v, V) else v


class K:
    def __init__(self, nc, es):
        self.nc = nc
        self.es = es
        self.s = Sched(nc)
        self.psum = []
        self.ps_i = 0
        self.dq = 0

    def tile(self, shape, dtype, name, es=None):
        self.dq += 1
        name = f"{name}_{self.dq}"
        h = (es or self.es).enter_context(self.nc.sbuf_tensor(name, list(shape), dtype))
        return Tile(h, name)

    def ptile(self, shape, dtype, name, es=None):
        h = (es or self.es).enter_context(self.nc.psum_tensor(name, list(shape), dtype))
        return Tile(h, name)

    def dram(self, name, shape, dtype, kind="Internal"):
        h = self.nc.dram_tensor(name, list(shape), dtype, kind=kind)
        t = Tile(h.ap(), name)
        t.buf.nowaw = True
        return t

    def ps(self):
        t = self.psum[self.ps_i % len(self.psum)]
        self.ps_i += 1
        return t

    def mm(self, out, lhsT, rhs, start=True, stop=True, **kw):
        self.s.add("pe", lambda E: E.matmul(out.ap, lhsT.ap, rhs.ap, start=start, stop=stop, **kw),
                   _bufs(lhsT, rhs), _bufs(out))

    def tr(self, out, in_, ident):
        self.s.add("pe", lambda E: E.transpose(out.ap, in_.ap, ident.ap), _bufs(in_, ident), _bufs(out))

    def act(self, out, in_, func, bias=None, scale=None, accum=None):
        kw = {}
        if bias is not None:
            kw["bias"] = _ap(bias)
        if scale is not None:
            kw["scale"] = _ap(scale)
        if accum is not None:
            kw["accum_out"] = accum.ap
        self.s.add("act", lambda E: E.activation(out.ap, in_.ap, func, **kw),
                   _bufs(in_, bias, scale), _bufs(out, accum))

    def tt(self, eng, out, in0, in1, op):
        self.s.add(eng, lambda E: E.tensor_tensor(out.ap, in0.ap, in1.ap, op), _bufs(in0, in1), _bufs(out))

    def ts(self, eng, out, in0, s1, op0, s2=None, op1=None, accum=None):
        kw = {}
        if op1 is not None:
            kw["op1"] = op1
        if accum is not None:
            kw["accum_out"] = accum.ap
        self.s.add(eng, lambda E: E.tensor_scalar(out.ap, in0.ap, _ap(s1), _ap(s2), op0, **kw),
                   _bufs(in0, s1, s2), _bufs(out, accum))

    def stt(self, eng, out, in0, scalar, in1, op0, op1):
        self.s.add(eng, lambda E: E.scalar_tensor_tensor(out.ap, in0.ap, _ap(scalar), in1.ap, op0, op1),
                   _bufs(in0, scalar, in1), _bufs(out))

    def copy(self, eng, out, in_):
        if eng == "act":
            self.s.add("act", lambda E: E.copy(out.ap, in_.ap), _bufs(in_), _bufs(out))
        else:
            self.s.add(eng, lambda E: E.tensor_copy(out.ap, in_.ap), _bufs(in_), _bufs(out))

    def memset(self, eng, out, val):
        self.s.add(eng, lambda E: E.memset(out.ap, val), [], _bufs(out))

    def recip(self, out, in_):
        self.s.add("dve", lambda E: E.reciprocal(out.ap, in_.ap), _bufs(in_), _bufs(out))

    def reduce(self, out, in_, op, axis=AX.X):
        self.s.add("dve", lambda E: E.tensor_reduce(out.ap, in_.ap, axis, op), _bufs(in_), _bufs(out))

    def max8(self, out, in_):
        self.s.add("dve", lambda E: E.max(out.ap, in_.ap), _bufs(in_), _bufs(out))

    def match_replace(self, out, to_replace, in_values, imm):
        self.s.add("dve", lambda E: E.match_replace(out.ap, to_replace.ap, in_values.ap, imm),
                   _bufs(to_replace, in_values), _bufs(out))

    def scan(self, out, d0, d1, initial, op0, op1):
        self.s.add("dve", lambda E: E.tensor_tensor_scan(out.ap, d0.ap, d1.ap, _ap(initial), op0, op1),
                   _bufs(d0, d1, initial), _bufs(out))

    def dma(self, q, out, in_, chan, **kw):
        pairs = list(zip(out, in_)) if isinstance(out, (list, tuple)) else [(out, in_)]
        rb = []
        wb = []
        for o, i in pairs:
            rb += _bufs(i)
            wb += _bufs(o)

        def fn(E):
            return [E.dma_start(out=o.ap, in_=i.ap, **kw) for o, i in pairs]
        self.s.add(q, fn, rb, wb, chan=chan, n=len(pairs))

    def emit(self):
        return self.s.emit(self.es)
from concourse.bass_utils import run_bass_kernel_spmd

NCORE = 8
S = 2048
NB = 2
T = NB * S
D = 1024
DIN = 7496
DFF = 2816
C_MQ, C_MK, C_MV = 0, 512, 1024
C_RW = 1536
RW_ROLES = dict(r=(0, 512), wd=(512, 64), k=(576, 512), v=(1088, 512), ad=(1600, 64), gd=(1664, 160))
C_DQ, C_DC, C_DIQ, C_DIK, C_DIW = 3360, 3872, 4128, 4384, 4416
C_GATE = 4424
NEG = -30000.0


def rel_bucket_np(dist):
    n = np.maximum(dist, 0)
    nf = np.maximum(n, 1).astype(np.float32)
    large = 16 + (np.log(nf / np.float32(16)) / np.float32(np.log(128 / 16)) * np.float32(16)).astype(np.int32)
    large = np.minimum(large, 31)
    return np.where(n < 16, n, large)


def pvec_keys():
    keys = []
    for j in range(4):
        keys.append(("qkg", j))
    for role, (off, n) in RW_ROLES.items():
        for c in range((n + 127) // 128):
            keys.append(("mu", role, c))
    for nm in ("w0", "a0", "kk", "ka", "rk", "v0"):
        for c in range(4):
            keys.append((nm, c))
    for c in range(2):
        keys.append(("kvn", c))
    return keys


PV_KEYS = pvec_keys()
PV_IDX = {k_: i for i, k_ in enumerate(PV_KEYS)}


def pvec_host(inp, l):
    out = np.zeros((128, len(PV_KEYS)), np.float32)
    for i, key in enumerate(PV_KEYS):
        if key[0] == "qkg":
            v = np.tile(inp["qk_norm"][l, key[1]], 2)
        elif key[0] == "mu":
            off, n = RW_ROLES[key[1]]
            c = key[2]
            v = inp["rwkv_mu"][l, off + c * 128: off + min(n, (c + 1) * 128)]
        elif key[0] == "kvn":
            v = inp["dsa_kv_norm"][l, key[1] * 128:(key[1] + 1) * 128]
        elif key[0] == "v0":
            v = inp["rwkv_v0"][0, key[1] * 128:(key[1] + 1) * 128]
        else:
            nm = dict(w0="rwkv_w0", a0="rwkv_a0", kk="rwkv_kk", ka="rwkv_ka", rk="rwkv_rk")[key[0]]
            v = inp[nm][l].reshape(512)[key[1] * 128:(key[1] + 1) * 128]
        out[:len(v), i] = v
    return out


def host_consts(inp):
    c = {}
    c["ident"] = np.eye(128, dtype=np.float32)
    b = np.zeros((128, 128), np.float32)
    b[:64, :64] = 1.0 / 64
    b[64:, 64:] = 1.0 / 64
    c["blk64"] = b
    c["pvec"] = np.stack([pvec_host(inp, l) for l in range(2)])
    kk_, qq_ = np.meshgrid(np.arange(128), np.arange(128), indexing="ij")
    bd = rel_bucket_np(qq_ - kk_)
    bn = rel_bucket_np(128 + qq_ - kk_)
    rb = inp["rel_bias"]
    relb = np.zeros((16, 2, 128, 128), np.float32)
    for h in range(16):
        relb[h, 0] = rb[bd, h]
        relb[h, 1] = rb[bn, h]
    c["relb"] = relb
    c["caus"] = np.where(qq_ >= kk_, 0.0, NEG).astype(np.float32)
    c["relfar"] = np.tile(rb[31:32, :], (128, 1)).astype(np.float32)
    c["causq"] = np.where(kk_ >= qq_, 0.0, -1e30).astype(np.float32)
    su = np.zeros((64, 3, 64), np.float32)
    ii, tt_ = np.meshgrid(np.arange(64), np.arange(64), indexing="ij")
    su[:, 0, :] = (ii < tt_)
    su[:, 1, :] = (ii <= tt_)
    su[:, 2, :] = (ii > tt_)
    c["umask"] = su
    return c


class Rot:
    def __init__(self, k, shape, dtype, name, n, es=None, init=None):
        self.tiles = [k.tile(shape, dtype, f"{name}{i}", es=es) for i in range(n)]
        self.i = 0
        if init is not None:
            for t in self.tiles:
                k.memset("dve", t[:], init)

    def next(self):
        t = self.tiles[self.i % len(self.tiles)]
        self.i += 1
        return t


def dv(t, ap, keys=None):
    return V(ap, [t.buf] if keys is None else t.subs_of(keys))


def build(dbg=None, phases=("P", "A", "R", "X", "M", "F"), layers=(0, 1), lim=None):
    dbg = dbg or {}
    lim = lim or {}
    nc = bass.Bass("TRN2", target_bir_lowering=False)
    es = ExitStack()
    with es:
        k = K(nc, es)

        def dram(name, shape, dtype, kind=None):
            return k.dram(name, shape, dtype, kind=kind or dbg.get(name, "Internal"))
        EI = "ExternalInput"
        x_in = dram("x", [T, D], F32, EI) if ("P" in phases or "M" in phases) else None
        w_in = dram("w_in", [2, D, DIN], F32, EI) if "P" in phases else None
        norm_mix = dram("norm_mix", [2, D], F32, EI)
        norm_ffn = dram("norm_ffn", [2, D], F32, EI)
        kv_up = dram("dsa_kv_up", [2, 256, 1024], F32, EI) if "P" in phases else None
        w_branch = dram("w_branch", [2, 3, 512, D], F32, EI) if "M" in phases else None
        w_o = dram("w_o", [2, D, D], F32, EI) if "M" in phases else None
        w_ffn_in = dram("w_ffn_in", [2, D, 2 * DFF], F32, EI) if "F" in phases else None
        w_ffn_out = dram("w_ffn_out", [2, DFF, D], F32, EI) if "F" in phases else None
        ident_d = dram("ident", [128, 128], F32, EI)
        blk64_d = dram("blk64", [128, 128], F32, EI)
        pvec_d = dram("pvec", [2, 128, len(PV_KEYS)], F32, EI)
        relb_d = dram("relb", [16, 2, 128, 128], F32, EI)
        caus_d = dram("caus", [128, 128], F32, EI)
        relfar_d = dram("relfar", [128, 16], F32, EI)
        causq_d = dram("causq", [128, 128], F32, EI)
        umask_d = dram("umask", [64, 3, 64], F32, EI)
        if "R" in phases:
            rw_w2 = dram("rwkv_w2", [2, 64, 512], F32, EI)
            rw_a2 = dram("rwkv_a2", [2, 64, 512], F32, EI)
            rw_g2 = dram("rwkv_g2", [2, 160, 512], F32, EI)
            rw_va = dram("rwkv_va", [1, 512, 32], F32, EI)
            rw_vb = dram("rwkv_vb", [1, 32, 512], F32, EI)
            rw_lnw = dram("rwkv_ln_w", [2, 512], F32, EI)
            rw_lnb = dram("rwkv_ln_b", [2, 512], F32, EI)
        y_out = dram("y", [T, D], F32, "ExternalOutput") if "F" in phases else None

        qkT = {nm: dram(nm, [4, 128, T], BF16) for nm in ("mqT", "mkT", "dqT", "dkT")}
        vE = {nm: dram(nm, [T, 8 * 65], BF16) for nm in ("mvE", "dvE")}
        rwT = {nm: dram("rw_" + nm, [4, 128, T], F32) for nm in ("r", "k", "v")}
        wdT = dram("rw_wd", [64, T], F32)
        adT = dram("rw_ad", [64, T], F32)
        gsT = dram("rw_gs", [2, 128, T], F32)
        qiT = dram("qiT", [2, 128, T], F32)
        kiT = dram("kiT", [128, T], F32)
        wiD = dram("wiD", [128, T // 128, 8], F32)
        sgT = dram("sgT", [24, 128, T], BF16)
        oT = {nm: dram("oT_" + nm, [4, 128, T], BF16) for nm in ("m", "r", "d")}
        vfirst = dram("vfirst", [4, 128, T], F32)
        xm_d = dram("xm", [T, D], F32)
        x1_d = dram("x1", [T, D], F32)

        for i in range(4):
            k.psum.append(k.ptile([128, 512], F32, f"ps{i}"))
        psR_t = [k.ptile([128, 512], F32, f"psR{i}") for i in range(4)]
        psR_i = [0]

        def psR():
            psR_i[0] += 1
            return psR_t[psR_i[0] % 4]

        idf = k.tile([128, 128], F32, "idf")
        idb = k.tile([128, 128], BF16, "idb")
        blkf = k.tile([128, 128], F32, "blkf")
        blkb = k.tile([128, 128], BF16, "blkb")
        onesb = k.tile([128, 128], BF16, "onesb")
        eps6 = k.tile([128, 1], F32, "eps6")
        pv = k.tile([128, 2, len(PV_KEYS)], F32, "pv")
        k.dma("sp", idf[:], ident_d[:], "c0")
        k.dma("sp", blkf[:], blk64_d[:], "c1")
        k.dma("sp", pv[:], dv(pvec_d, pvec_d.h.rearrange("l p n -> p l n")), "c2")
        k.copy("dve", idb[:], idf[:])
        k.copy("dve", blkb[:], blkf[:])
        k.memset("dve", onesb[:], 1.0 / 256)
        k.memset("dve", eps6[:], 1e-6)

        def pvc(l, *key, m=128):
            i = PV_IDX[tuple(key)]
            return pv[0:m, l, i:i + 1]

        def pbf(p):
            return V(p.h[:].bitcast(BF16), [p.buf])

        def norm_transpose(pes_outer, src, gain_row, hT, tok0, ntile, tag):
          with ExitStack() as pes:
            gb = k.tile([128, D], F32, "gb" + tag, es=pes)
            k.dma("sp", gb[:], dv(gain_row[0], gain_row[1].partition_broadcast(128)), "c3")
            xr = Rot(k, [128, D], F32, "xr" + tag, 2, es=pes)
            sqr = Rot(k, [128, D], BF16, "sqr" + tag, 2, es=pes)
            ssr = Rot(k, [128, 1], F32, "ssr" + tag, 2, es=pes)
            hbr = Rot(k, [128, D], BF16, "hbr" + tag, 2, es=pes)
            for t in range(ntile):
                xt, sq, ss, hb = xr.next(), sqr.next(), ssr.next(), hbr.next()
                r0 = tok0 + t * 128
                k.dma("sp", xt[:], src[r0:r0 + 128, :], f"xl{t % 2}")
                k.act(sq[:], xt[:], AF.Square, accum=ss[:])
                k.act(ss[:], ss[:], AF.Sqrt, bias=eps6[:], scale=1.0 / D)
                k.recip(ss[:], ss[:])
                k.stt("dve", hb[:], xt[:], ss[:], gb[:], ALU.mult, ALU.mult)
                p = k.ps()
                pb = pbf(p)
                for kc in range(8):
                    k.tr(V(pb.ap[:, kc * 128:(kc + 1) * 128], pb.bufs), hb[:, kc * 128:(kc + 1) * 128], idb[:])
                k.copy("act", hT[:, :, t * 128:(t + 1) * 128],
                       V(pb.ap[:, 0:1024].rearrange("p (k t) -> p k t", k=8), pb.bufs))
          k.s.barrier()

        wq = [0]

        def load_w(wt, src_ap, src_t, nk, n):
            wq[0] += 1
            k.dma("pool", wt[:, 0:nk, 0:n], dv(src_t, src_ap.rearrange("(kc p) c -> p kc c", p=128)),
                  f"w{wq[0] % 2}")

        def gemm_fm(wt, c0, n, hT, t0, nk):
            p = k.ps()
            for kc in range(nk):
                k.mm(p[0:n, :], wt[:, kc, c0:c0 + n], hT[:, kc, t0:t0 + 512], start=(kc == 0), stop=(kc == nk - 1))
            return p

        def gemm_tm(wt, n, hT, t0, nk):
            p = k.ps()
            for kc in range(nk):
                k.mm(p[:, 0:n], hT[:, kc, t0:t0 + 128], wt[:, kc, 0:n], start=(kc == 0), stop=(kc == nk - 1))
            return p

        for l in layers:
            xsrc = x_in if l == 0 else x1_d
            xdst = x1_d if l == 0 else y_out
            if "P" in phases:
                k.s.barrier()
                with ExitStack() as pes:
                    hT = k.tile([128, 8, T], BF16, "hT", es=pes)
                    cnT = k.tile([128, 2, T], BF16, "cnT", es=pes)
                    norm_transpose(pes, xsrc, (norm_mix, norm_mix.h[l:l + 1, :]), hT, 0, T // 128, "p")
                    wr = Rot(k, [128, 8, 512], BF16, "wt", 2, es=pes)
                    qfr = Rot(k, [128, 512], F32, "qf", 3, es=pes)
                    sqr = Rot(k, [128, 512], BF16, "sq", 3, es=pes)
                    sdr = Rot(k, [128, 512], F32, "sd", 3, es=pes)
                    obr = Rot(k, [128, 512], BF16, "ob", 3, es=pes)
                    ver = Rot(k, [128, 8, 65], BF16, "ve", 3, es=pes, init=1.0)
                    ofr = Rot(k, [128, 512], F32, "of", 3, es=pes)
                    oq = [0]

                    def dout(dst_v, src_v):
                        oq[0] += 1
                        k.dma("sp", dst_v, src_v, f"o{oq[0] % 4}")

                    def qk_fm(wt, hsrc, nk, gkey, dst):
                        for c in range(4):
                            for tb in range(T // 512):
                                p = gemm_fm(wt, c * 128, 128, hsrc, tb * 512, nk)
                                qf, sq, sd, ob = qfr.next(), sqr.next(), sdr.next(), obr.next()
                                k.act(qf[:], p[:], AF.Copy)
                                k.act(sq[:], p[:], AF.Square)
                                p2 = k.ps()
                                k.mm(p2[:], blkb[:], sq[:])
                                k.act(sd[:], p2[:], AF.Sqrt, bias=eps6[:])
                                k.recip(sd[:], sd[:])
                                k.stt("dve", ob[:], qf[:], pvc(l, "qkg", gkey), sd[:], ALU.mult, ALU.mult)
                                dout(dv(dst, dst.h[c, :, tb * 512:(tb + 1) * 512]), ob[:])

                    def v_tm(wt, hsrc, nk, dst):
                        for tt in range(T // 128):
                            p = gemm_tm(wt, 512, hsrc, tt * 128, nk)
                            ve = ver.next()
                            k.copy("act" if tt % 2 else "dve", ve[:, :, 0:64],
                                   V(p.h[:, :].rearrange("p (h d) -> p h d", h=8), [p.buf]))
                            dout(dv(dst, dst.h[tt * 128:(tt + 1) * 128, :]),
                                 V(ve.h[:].rearrange("p h d -> p (h d)"), [ve.buf]))

                    for (c0, gk, dst) in ((C_MQ, 0, qkT["mqT"]), (C_MK, 1, qkT["mkT"]), (C_DQ, 2, qkT["dqT"])):
                        wt = wr.next()
                        load_w(wt, w_in.h[l, :, c0:c0 + 512], w_in, 8, 512)
                        qk_fm(wt, hT, 8, gk, dst)
                    wt = wr.next()
                    load_w(wt, w_in.h[l, :, C_MV:C_MV + 512], w_in, 8, 512)
                    v_tm(wt, hT, 8, vE["mvE"])
                    wt = wr.next()
                    load_w(wt, w_in.h[l, :, C_DC:C_DC + 256], w_in, 8, 256)
                    for tb in range(T // 512):
                        ps_c = [gemm_fm(wt, c * 128, 128, hT, tb * 512, 8) for c in range(2)]
                        cfs, sqs = [], []
                        for c in range(2):
                            cf, sq = qfr.next(), sqr.next()
                            k.act(cf[:], ps_c[c][:], AF.Copy)
                            k.act(sq[:], ps_c[c][:], AF.Square)
                            cfs.append(cf)
                            sqs.append(sq)
                        p2 = k.ps()
                        k.mm(p2[:], onesb[:], sqs[0][:], start=True, stop=False)
                        k.mm(p2[:], onesb[:], sqs[1][:], start=False, stop=True)
                        sd = sdr.next()
                        k.act(sd[:], p2[:], AF.Sqrt, bias=eps6[:])
                        k.recip(sd[:], sd[:])
                        for c in range(2):
                            k.stt("dve", cnT[:, c, tb * 512:(tb + 1) * 512], cfs[c][:], pvc(l, "kvn", c), sd[:],
                                  ALU.mult, ALU.mult)
                    wt = wr.next()
                    load_w(wt, kv_up.h[l, :, 0:512], kv_up, 2, 512)
                    qk_fm(wt, cnT, 2, 3, qkT["dkT"])
                    wt = wr.next()
                    load_w(wt, kv_up.h[l, :, 512:1024], kv_up, 2, 512)
                    v_tm(wt, cnT, 2, vE["dvE"])
                    wt = wr.next()
                    load_w(wt, w_in.h[l, :, C_DIQ:C_DIQ + 256], w_in, 8, 256)
                    for c in range(2):
                        for tb in range(T // 512):
                            p = gemm_fm(wt, c * 128, 128, hT, tb * 512, 8)
                            of = ofr.next()
                            k.copy("act", of[:], p[:])
                            dout(dv(qiT, qiT.h[c, :, tb * 512:(tb + 1) * 512]), of[:])
                    wt = wr.next()
                    for r4 in range(4):
                        wq[0] += 1
                        k.dma("pool", wt[:, 0:8, r4 * 32:(r4 + 1) * 32],
                              dv(w_in, w_in.h[l, :, C_DIK:C_DIK + 32].rearrange("(kc p) c -> p kc c", p=128)),
                              f"w{wq[0] % 2}")
                    wq[0] += 1
                    k.dma("pool", wt[:, 0:8, 128:136],
                          dv(w_in, w_in.h[l, :, C_DIW:C_DIW + 8].rearrange("(kc p) c -> p kc c", p=128)),
                          f"w{wq[0] % 2}")
                    for tb in range(T // 512):
                        p = gemm_fm(wt, 0, 128, hT, tb * 512, 8)
                        of = ofr.next()
                        k.copy("act", of[:], p[:])
                        dout(dv(kiT, kiT.h[:, tb * 512:(tb + 1) * 512]), of[:])
                    for tt in range(T // 128):
                        p = k.ps()
                        for kc in range(8):
                            k.mm(p[:, 0:8], hT[:, kc, tt * 128:(tt + 1) * 128], wt[:, kc, 128:136],
                                 start=(kc == 0), stop=(kc == 7))
                        of = ofr.next()
                        k.copy("dve", of[:, 0:8], p[:, 0:8])
                        dout(dv(wiD, wiD.h[:, tt, :]), of[:, 0:8])
                    for g6 in range(6):
                        wt = wr.next()
                        load_w(wt, w_in.h[l, :, C_GATE + g6 * 512:C_GATE + (g6 + 1) * 512], w_in, 8, 512)
                        for c in range(4):
                            for tb in range(T // 512):
                                p = gemm_fm(wt, c * 128, 128, hT, tb * 512, 8)
                                ob = obr.next()
                                k.act(ob[:], p[:], AF.Sigmoid)
                                dout(dv(sgT, sgT.h[g6 * 4 + c, :, tb * 512:(tb + 1) * 512]), ob[:])
                    pfr = Rot(k, [128, S + 1], F32, "pf", 2, es=pes, init=0.0)
                    ddr = Rot(k, [128, S], F32, "dd", 2, es=pes)
                    mxr = Rot(k, [128, S], F32, "mx", 2, es=pes)
                    for role, (off, n) in RW_ROLES.items():
                        wt = wr.next()
                        load_w(wt, w_in.h[l, :, C_RW + off:C_RW + off + n], w_in, 8, n)
                        for c in range((n + 127) // 128):
                            m = min(128, n - c * 128)
                            for b in range(NB):
                                pf, dd, mx = pfr.next(), ddr.next(), mxr.next()
                                for tbl in range(4):
                                    p = gemm_fm(wt, c * 128, m, hT, b * S + tbl * 512, 8)
                                    k.copy("act", pf[0:m, 1 + tbl * 512:1 + (tbl + 1) * 512], p[0:m, :])
                                k.tt("dve", dd[0:m, :], pf[0:m, 0:S], pf[0:m, 1:S + 1], ALU.subtract)
                                k.stt("dve", mx[0:m, :], dd[0:m, :], pvc(l, "mu", role, c, m=m), pf[0:m, 1:S + 1],
                                      ALU.mult, ALU.add)
                                tsl = slice(b * S, (b + 1) * S)
                                if role in ("r", "k", "v"):
                                    dout(dv(rwT[role], rwT[role].h[c, :, tsl]), mx[:])
                                    if role == "v" and l == 0:
                                        dout(dv(vfirst, vfirst.h[c, :, tsl]), mx[:])
                                elif role == "wd":
                                    k.act(mx[0:64, :], mx[0:64, :], AF.Tanh)
                                    dout(dv(wdT, wdT.h[:, tsl]), mx[0:64, :])
                                elif role == "ad":
                                    dout(dv(adT, adT.h[:, tsl]), mx[0:64, :])
                                else:
                                    k.act(mx[0:m, :], mx[0:m, :], AF.Sigmoid)
                                    dout(dv(gsT, gsT.h[c, 0:m, tsl]), mx[0:m, :])


            def attention(kind, pes):
                hoff = 0 if kind == "m" else 8
                qd, kd, vd = (qkT["mqT"], qkT["mkT"], vE["mvE"]) if kind == "m" else (qkT["dqT"], qkT["dkT"], vE["dvE"])
                od = oT[kind]
                if True:
                    bt = k.tile([128, 8, 2, 128], F32, "bt", es=pes)
                    far = k.tile([128, 16], F32, "far", es=pes)
                    caus = k.tile([128, 128], F32, "caus", es=pes)
                    k.dma("sp", bt[:], dv(relb_d, relb_d.h[hoff:hoff + 8].rearrange("h t k q -> k h t q")), kind + "al0")
                    k.dma("sp", far[:], relfar_d[:], kind + "al1")
                    k.dma("sp", caus[:], caus_d[:], kind + "al2")
                    for h in range(8):
                        k.tt("dve", bt[:, h, 0, :], bt[:, h, 0, :], caus[:], ALU.add)
                    qT = k.tile([128, 4, S], BF16, "aqT", es=pes)
                    kT = k.tile([128, 4, S], BF16, "akT", es=pes)
                    vt = k.tile([128, 16, 520], BF16, "avt", es=pes)
                    Er = Rot(k, [128, 128], BF16, "E", 6, es=pes)
                    tmr = Rot(k, [128, 128], F32, "tm", 3, es=pes)
                    accr = Rot(k, [128, 65], F32, "acc", 6, es=pes)
                    rcr = Rot(k, [128, 1], F32, "rc", 6, es=pes)
                    otr = Rot(k, [128, 512], BF16, "otl", 2, es=pes)
                    oTr = Rot(k, [128, 4, 128], BF16, "oTt", 2, es=pes)
                    if kind == "m":
                        ks = k.tile([128, 4, 8], F32, "ks", es=pes)
                        kmh = k.tile([128, 4, 8], BF16, "kmh", es=pes)
                        kml = k.tile([128, 4, 8], BF16, "kml", es=pes)
                        kd_ = k.tile([128, 4, 8], F32, "kd_", es=pes)
                        pm = k.tile([128, 8, 64], F32, "pm", es=pes)
                        k.memset("dve", pm[:], 0.0)
                        for ni in range(8):
                            k.memset("dve", V(pm.h[:, ni, :].rearrange("p (h n) -> p h n", n=8)[:, :, ni:8], [pm.buf]), -1e30)
                        gr = Rot(k, [128, 64], F32, "gat", 2, es=pes)
                        cmr = Rot(k, [128, 512], F32, "cmp", 1, es=pes)
                        cnr = Rot(k, [128, 64], F32, "cnt", 2, es=pes)
                        mkr = Rot(k, [128, 64], F32, "msk", 2, es=pes)
                    else:
                        qi = k.tile([128, 3, S], F32, "qi", es=pes)
                        ki = k.tile([128, S], F32, "ki", es=pes)
                        wi = k.tile([128, 16, 8], F32, "wi", es=pes)
                        wa = k.tile([128, 16, 8], F32, "wa", es=pes)
                        wsg = k.tile([128, 16, 8], F32, "wsg", es=pes)
                        cq = k.tile([128, 128], F32, "cq", es=pes)
                        k.dma("sp", cq[:], causq_d[:], kind + "al3")
                        Isc = k.tile([128, S], F32, "Isc", es=pes)
                        wk = k.tile([128, S], F32, "wk", es=pes)
                        rrr = Rot(k, [128, 512], F32, "rr", 2, es=pes)
                        m8r = Rot(k, [128, 8], F32, "m8", 2, es=pes)
                        mskb = k.tile([128, S], BF16, "mskb", es=pes)
                        mTr = Rot(k, [128, 16, 128], BF16, "mT", 1, es=pes)
                        Emr = Rot(k, [128, 128], BF16, "Em", 6, es=pes)
                    for b in range(lim.get("nb", NB)):
                        tsl = slice(b * S, (b + 1) * S)
                        k.dma("sp", qT[:], dv(qd, qd.h[:, :, tsl].rearrange("c p t -> p c t")), kind + "al0")
                        k.dma("sp", kT[:], dv(kd, kd.h[:, :, tsl].rearrange("c p t -> p c t")), kind + "al1")
                        k.dma("sp", vt[:], dv(vd, vd.h[tsl, :].rearrange("(j p) f -> p j f", p=128)), kind + "al2")
                        if kind == "m":
                            k.reduce(ks[:], V(kT.h[:].rearrange("p c (n s) -> p c n s", s=256), [kT.buf]), ALU.add)
                            k.ts("dve", kd_[:], ks[:], 1.0 / 256, ALU.mult)
                            k.copy("dve", kmh[:], kd_[:])
                            k.tt("dve", kd_[:], kd_[:], kmh[:], ALU.subtract)
                            k.copy("dve", kml[:], kd_[:])
                        else:
                            k.dma("sp", [qi[32 * (h_ % 3):32 * (h_ % 3) + 32, h_ // 3, :] for h_ in range(8)],
                                  [dv(qiT, qiT.h[h_ // 4, 32 * (h_ % 4):32 * (h_ % 4) + 32, tsl]) for h_ in range(8)], kind + "al3")
                            k.dma("sp", ki[:], kiT[:, tsl], kind + "al4")
                            k.dma("sp", wi[:], dv(wiD, wiD.h[:, b * 16:(b + 1) * 16, :]), kind + "al5")
                            k.act(wa[:], wi[:], AF.Abs)
                            k.act(wsg[:], wi[:], AF.Sign)
                        for i in range(lim.get("qt", 16)):
                            qs = slice(i * 128, (i + 1) * 128)
                            ni = i // 2
                            if kind == "m":
                                g, cm, cn, msk = gr.next(), cmr.next(), cnr.next(), mkr.next()
                                for par in range(2):
                                    pg = k.ps()
                                    first = True
                                    base = 64 * par
                                    for c in range(4):
                                        for km in (kmh, kml):
                                            k.mm(pg[:, c * 8:(c + 1) * 8], qT[base:base + 64, c, qs], km[base:base + 64, c, :],
                                                 start=first, stop=True, skip_group_check=True)
                                            first = False
                                    k.tt("dve", V(g.h[:, :].rearrange("p (c par n) -> p c par n", par=2, n=8)[:, :, par, :], [g.buf]),
                                         V(pg.h[:, 0:32].rearrange("p (c n) -> p c n", n=8), [pg.buf]),
                                         V(pm.h[:, ni, :].rearrange("p (c par n) -> p c par n", par=2, n=8)[:, :, par, :], [pm.buf]), ALU.add)
                                g3 = g.h[:, :].rearrange("p (h n) -> p h n", n=8)
                                k.tt("dve", V(cm.h[:, :].rearrange("p (h n m) -> p h n m", n=8, m=8), [cm.buf]),
                                     V(g3.unsqueeze(2).broadcast_to([128, 8, 8, 8]), [g.buf]),
                                     V(g3.unsqueeze(3).broadcast_to([128, 8, 8, 8]), [g.buf]), ALU.is_gt)
                                k.reduce(cn[:], V(cm.h[:, :].rearrange("p (hn m) -> p hn m", m=8), [cm.buf]), ALU.add)
                                k.ts("dve", msk[:], cn[:], 3.0, ALU.is_lt)
                            else:
                                n = 128 * (i + 1)
                                for kb in range((n + 511) // 512):
                                    wdt = min(512, n - kb * 512)
                                    for h in range(8):
                                        pb_, cq_ = 32 * (h % 3), h // 3
                                        p = k.ps()
                                        k.mm(p[:, 0:wdt], qi[pb_:pb_ + 32, cq_, qs], ki[pb_:pb_ + 32, kb * 512:kb * 512 + wdt])
                                        rr = rrr.next()
                                        k.act(rr[:, 0:wdt], p[:, 0:wdt], AF.Relu, scale=wa[:, i, h:h + 1])
                                        if h == 0:
                                            k.ts("dve", Isc[:, kb * 512:kb * 512 + wdt], rr[:, 0:wdt], wsg[:, i, 0:1], ALU.mult)
                                        else:
                                            k.stt("dve", Isc[:, kb * 512:kb * 512 + wdt], rr[:, 0:wdt], wsg[:, i, h:h + 1],
                                                  Isc[:, kb * 512:kb * 512 + wdt], ALU.mult, ALU.add)
                                k.tt("dve", Isc[:, qs], Isc[:, qs], cq[:], ALU.add)
                                if i >= 2:
                                    k.copy("act", wk[:, 0:n], Isc[:, 0:n])
                                    for rd in range(32):
                                        m8 = m8r.next()
                                        k.max8(m8[:], wk[:, 0:n])
                                        if rd < 31:
                                            k.match_replace(wk[:, 0:n], m8[:], wk[:, 0:n], -1e30)
                                    k.ts("dve", mskb[:, 0:n], Isc[:, 0:n], m8[:, 7:8], ALU.is_ge)
                                else:
                                    k.ts("dve", mskb[:, 0:n], Isc[:, 0:n], -1e29, ALU.is_gt)
                                mT = mTr.next()
                                for j0 in range(0, i + 1, 8):
                                    p = k.ps()
                                    pb = pbf(p)
                                    nj = min(8, i + 1 - j0)
                                    for jj in range(nj):
                                        k.tr(V(pb.ap[:, jj * 128:(jj + 1) * 128], pb.bufs), mskb[:, (j0 + jj) * 128:(j0 + jj + 1) * 128], idb[:])
                                    k.copy("act", mT[:, j0:j0 + nj, :],
                                           V(pb.ap[:, 0:nj * 128].rearrange("p (j t) -> p j t", t=128), pb.bufs))
                            ot = otr.next()
                            for h in range(8):
                                c, base = h // 2, 64 * (h % 2)
                                fb = far[:, hoff + h:hoff + h + 1]

                                def make_E(j):
                                    ps_ = k.ps()
                                    k.mm(ps_[:, 0:128], kT[base:base + 64, c, j * 128:(j + 1) * 128], qT[base:base + 64, c, qs])
                                    E = Er.next()
                                    d_ = i - j
                                    if d_ >= 2:
                                        k.act(E[:], ps_[:, 0:128], AF.Exp, bias=fb, scale=0.125)
                                    else:
                                        tm = tmr.next()
                                        k.stt("dve", tm[:], ps_[:, 0:128], 0.125, bt[:, h, d_, :], ALU.mult, ALU.add)
                                        k.act(E[:], tm[:], AF.Exp)
                                    return E
                                acc = accr.next()
                                if kind == "m":
                                    for nblk in range(ni + 1):
                                        js = [j for j in (2 * nblk, 2 * nblk + 1) if j <= i]
                                        R = psR()
                                        for jx, j in enumerate(js):
                                            E = make_E(j)
                                            k.mm(R[:, 0:65], E[:], vt[:, j, h * 65:(h + 1) * 65], start=(jx == 0), stop=(jx == len(js) - 1))
                                        mcol = msk[:, h * 8 + nblk:h * 8 + nblk + 1]
                                        if nblk == 0:
                                            if nblk < ni:
                                                k.ts("dve", acc[:], R[:, 0:65], mcol, ALU.mult)
                                            else:
                                                k.copy("dve", acc[:], R[:, 0:65])
                                        elif nblk < ni:
                                            k.stt("dve", acc[:], R[:, 0:65], mcol, acc[:], ALU.mult, ALU.add)
                                        else:
                                            k.tt("dve", acc[:], R[:, 0:65], acc[:], ALU.add)
                                else:
                                    R = psR()
                                    for j in range(i + 1):
                                        E = make_E(j)
                                        Em = Emr.next()
                                        k.tt("dve", Em[:], E[:], mT[:, j, :], ALU.mult)
                                        k.mm(R[:, 0:65], Em[:], vt[:, j, h * 65:(h + 1) * 65], start=(j == 0), stop=(j == i))
                                    k.copy("dve", acc[:], R[:, 0:65])
                                rc = rcr.next()
                                k.recip(rc[:], acc[:, 64:65])
                                k.ts("dve", ot[:, h * 64:(h + 1) * 64], acc[:, 0:64], rc[:], ALU.mult)
                                yield
                            p = k.ps()
                            pb = pbf(p)
                            for c4 in range(4):
                                k.tr(V(pb.ap[:, c4 * 128:(c4 + 1) * 128], pb.bufs), ot[:, c4 * 128:(c4 + 1) * 128], idb[:])
                            oTt = oTr.next()
                            k.copy("act", oTt[:], V(pb.ap[:, 0:512].rearrange("p (c t) -> p c t", c=4), pb.bufs))
                            k.dma("sp", dv(od, od.h[:, :, b * S + i * 128:b * S + (i + 1) * 128].rearrange("c p t -> p c t")), oTt[:], kind + f"ao{i % 2}")

            kinds = [kd_ for kd_, ph_ in (("m", "A"), ("d", "X")) if ph_ in phases]
            if kinds:
                k.s.barrier()
                with ExitStack() as pes_ax:
                    gens = [attention(kd_, pes_ax) for kd_ in kinds]
                    while gens:
                        for g_ in list(gens):
                            try:
                                next(g_)
                            except StopIteration:
                                gens.remove(g_)


            if "R" in phases:
                C0 = float(np.exp(-0.5))
                BT = 256
                NCH = BT // 64
                k.s.barrier()
                with ExitStack() as pes:
                    um = k.tile([64, 3, 64], F32, "um", es=pes)
                    k.dma("sp", um[:], umask_d[:], "rl0")
                    w2s = k.tile([128, 512], F32, "w2s", es=pes)
                    a2s = k.tile([128, 512], F32, "a2s", es=pes)
                    g2s = k.tile([128, 2, 512], BF16, "g2s", es=pes)
                    k.memset("dve", w2s[:], 0.0)
                    k.memset("dve", a2s[:], 0.0)
                    k.dma("sp", w2s[0:64, :], rw_w2[l, :, :], "rl1")
                    k.dma("sp", a2s[0:64, :], rw_a2[l, :, :], "rl2")
                    k.dma("pool", g2s[:, 0, :], rw_g2[l, 0:128, :], "pg0")
                    k.dma("pool", g2s[0:32, 1, :], rw_g2[l, 128:160, :], "pg1")
                    lnw = k.tile([64, 512], F32, "lnw", es=pes)
                    lnb = k.tile([64, 512], F32, "lnb", es=pes)
                    k.dma("sp", lnw[:], dv(rw_lnw, rw_lnw.h[l:l + 1, :].partition_broadcast(64)), "rl5")
                    k.dma("sp", lnb[:], dv(rw_lnb, rw_lnb.h[l:l + 1, :].partition_broadcast(64)), "rl6")
                    if l == 1:
                        vas = k.tile([128, 4, 128], F32, "vas", es=pes)
                        k.memset("dve", vas[:], 0.0)
                        vbs = k.tile([32, 512], F32, "vbs", es=pes)
                        k.dma("sp", vas[:, :, 0:32], dv(rw_va, rw_va.h[0].rearrange("(c p) r -> p c r", p=128)), "rl7")
                        k.dma("sp", vbs[:], rw_vb[0, :, :], "rl8")
                    ind2 = k.tile([128, 2], BF16, "ind2", es=pes)
                    k.memset("dve", ind2[:], 0.0)
                    k.memset("dve", ind2[0:64, 0:1], 1.0)
                    k.memset("dve", ind2[64:128, 1:2], 1.0)
                    rmask = k.tile([128, BT], F32, "rmask", es=pes)
                    k.memset("dve", rmask[:], 1.0)
                    k.memset("dve", V(rmask.h[:, :].rearrange("p (c t) -> p c t", t=64)[:, :, 0:1], [rmask.buf]), 0.0)
                    omka = k.tile([128, 4], F32, "omka", es=pes)
                    for c in range(4):
                        k.ts("dve", omka[:, c:c + 1], pvc(l, "ka", c), -1.0, ALU.mult, 1.0, ALU.add)
                    epsg = k.tile([128, 1], F32, "epsg", es=pes)
                    k.memset("dve", epsg[:], 64e-5)
                    epsk = k.tile([128, 1], F32, "epsk", es=pes)
                    k.memset("dve", epsk[:], 1e-24)

                    def ft(name, n=1):
                        return Rot(k, [128, 4, BT], F32, name, n, es=pes)
                    rTr, kTr_, vTr_, vfr = ft("rT"), ft("kTl"), ft("vTl"), ft("vfl", 1)
                    wdr = Rot(k, [128, BT], F32, "wdl", 1, es=pes, init=0.0)
                    adr = Rot(k, [128, BT], F32, "adl", 1, es=pes, init=0.0)
                    gsr = Rot(k, [128, 2, BT], BF16, "gsl", 1, es=pes)
                    sgw, aa, Ls, G_, Gi, Gp = ft("sgw", 1), ft("aa", 1), ft("Ls", 1), ft("G_", 1), ft("Gi", 1), ft("Gp", 1)
                    kkn, t1f, t2f = ft("kkn", 1), ft("t1f", 1), ft("t2f", 1)
                    ARer = Rot(k, [128, 4, NCH, 2, 64], BF16, "ARe", 1, es=pes, init=0.0)
                    ARor = Rot(k, [128, 4, NCH, 2, 64], BF16, "ARo", 1, es=pes, init=0.0)
                    def fb_(name):
                        return Rot(k, [128, 4, BT], BF16, name, 1, es=pes)
                    bhbr, khbr, btbr, ktbr, vbr, prbr = fb_("bhb"), fb_("khb"), fb_("btb"), fb_("ktb"), fb_("vbb"), fb_("prb")
                    Hbr = Rot(k, [128, 4, 64], BF16, "Hb", 2, es=pes)

                    GCr = Rot(k, [128, 4, NCH], F32, "GC", 2, es=pes)
                    tokr = {nm: Rot(k, [64, 512], BF16, "tk" + nm, 3, es=pes) for nm in ("b", "k", "v")}
                    m13r = Rot(k, [64, 8, 2, 64], BF16, "m13", 3, es=pes)
                    m24r = Rot(k, [64, 8, 2, 64], BF16, "m24", 3, es=pes)
                    sq_ = {nm: Rot(k, [64, 8, 64], BF16, "q" + nm, (14 if nm == "P" else 3), es=pes) for nm in ("M", "N", "P", "Q")}
                    xsr = Rot(k, [64, 512], BF16, "xs", 2, es=pes)
                    usr = Rot(k, [64, 512], BF16, "us", 2, es=pes)
                    osr = Rot(k, [64, 512], F32, "os", 2, es=pes)
                    Hr = Rot(k, [128, 4, 64], F32, "H", 2, es=pes)
                    s8r = Rot(k, [64, 8], F32, "s8", 6, es=pes)
                    cer = Rot(k, [64, 512], F32, "ce", 2, es=pes)
                    sqr2 = Rot(k, [64, 512], F32, "sq2", 2, es=pes)
                    ybr = Rot(k, [64, 512], BF16, "yb", 2, es=pes)
                    oTr2 = Rot(k, [128, 4, BT], BF16, "oTr", 2, es=pes)
                    t1s = k.tile([32, BT], F32, "t1s", es=pes)
                    evi = [0]

                    def evac(dst, src):
                        evi[0] += 1
                        k.copy("act" if evi[0] % 2 else "dve", dst, src)

                    def flat(t):
                        return V(t.h[:].rearrange("p c t -> p (c t)"), [t.buf])

                    def hv(t, h):
                        return t[:, h, :]

                    for b in range(lim.get("nb", NB)):
                        H = Hr.next()
                        k.memset("dve", H[:], 0.0)
                        Hb = Hbr.next()
                        k.memset("dve", Hb[:], 0.0)
                        for tb in range(lim.get("rb", S // BT)):
                            t0 = b * S + tb * BT
                            tsl = slice(t0, t0 + BT)
                            rT, kTl, vTl = rTr.next(), kTr_.next(), vTr_.next()
                            wdl, adl, gsl = wdr.next(), adr.next(), gsr.next()
                            k.dma("sp", rT[:], dv(rwT["r"], rwT["r"].h[:, :, tsl].rearrange("c p t -> p c t")), "rl0")
                            k.dma("sp", kTl[:], dv(rwT["k"], rwT["k"].h[:, :, tsl].rearrange("c p t -> p c t")), "rl1")
                            k.dma("sp", vTl[:], dv(rwT["v"], rwT["v"].h[:, :, tsl].rearrange("c p t -> p c t")), "rl2")
                            k.dma("sp", wdl[0:64, :], wdT[:, tsl], "rl3")
                            k.dma("sp", adl[0:64, :], adT[:, tsl], "rl4")
                            k.dma("pool", gsl[:], dv(gsT, gsT.h[:, :, tsl].rearrange("c p t -> p c t")), "pg2")
                            sg_, a_, L_, G, GI, GP = sgw.next(), aa.next(), Ls.next(), G_.next(), Gi.next(), Gp.next()
                            kn, t1, t2 = kkn.next(), t1f.next(), t2f.next()
                            for c in range(4):
                                cs = slice(c * 128, (c + 1) * 128)
                                p = k.ps()
                                k.mm(p[:, 0:BT], w2s[:, cs], wdl[:, :])
                                k.act(sg_[:, c, :], p[:, 0:BT], AF.Sigmoid, bias=pvc(l, "w0", c))
                                p = k.ps()
                                k.mm(p[:, 0:BT], a2s[:, cs], adl[:, :])
                                k.act(a_[:, c, :], p[:, 0:BT], AF.Sigmoid, bias=pvc(l, "a0", c))
                            if l == 1:
                                vf = vfr.next()
                                k.dma("sp", vf[:], dv(vfirst, vfirst.h[:, :, tsl].rearrange("c p t -> p c t")), "rl6")
                                p = k.ps()
                                for c in range(4):
                                    k.mm(p[:, 0:BT], vas[:, c, :], vTl[:, c, :], start=(c == 0), stop=(c == 3))
                                k.copy("act", t1s[:], p[0:32, 0:BT])
                                for c in range(4):
                                    p = k.ps()
                                    k.mm(p[:, 0:BT], vbs[0:32, c * 128:(c + 1) * 128], t1s[0:32, :])
                                    k.act(t1[:, c, :], p[:, 0:BT], AF.Sigmoid, bias=pvc(l, "v0", c))
                                k.tt("dve", flat(vf), flat(vf), flat(vTl), ALU.subtract)
                                k.tt("dve", flat(vf), flat(vf), flat(t1), ALU.mult)
                                k.tt("dve", flat(vTl), flat(vTl), flat(vf), ALU.add)
                            for c in range(4):
                                k.ts("dve", kn[:, c, :], kTl[:, c, :], pvc(l, "kk", c), ALU.mult)
                            k.tt("dve", flat(t1), flat(kn), flat(kn), ALU.mult)
                            for c in range(4):
                                p = k.ps()
                                k.mm(p[:, 0:BT], blkf[:], t1[:, c, :])
                                k.act(t2[:, c, :], p[:, 0:BT], AF.Sqrt, bias=epsk[:], scale=64.0)
                            k.recip(flat(t2), flat(t2))
                            k.tt("dve", flat(kn), flat(kn), flat(t2), ALU.mult)
                            for c in range(4):
                                k.ts("dve", t1[:, c, :], a_[:, c, :], pvc(l, "ka", c), ALU.mult, omka[:, c:c + 1], ALU.add)
                            k.tt("dve", flat(kTl), flat(kTl), flat(t1), ALU.mult)
                            for c in range(4):
                                k.scan(L_[:, c, :], rmask[:], sg_[:, c, :], 0.0, ALU.mult, ALU.add)
                            k.act(flat(G), flat(L_), AF.Exp, scale=-C0)
                            k.act(flat(GI), flat(L_), AF.Exp, scale=C0)
                            k.tt("dve", flat(t2), flat(L_), flat(sg_), ALU.subtract)
                            k.act(flat(GP), flat(t2), AF.Exp, scale=-C0)
                            GC = GCr.next()
                            k.copy("dve", GC[:], V(G.h[:].rearrange("p c (ch t) -> p c ch t", t=64)[:, :, :, 63], [G.buf]))
                            ARx = (ARer.next(), ARor.next())
                            bh, kh, bt2, kt2 = t1, t2, sg_, a_

                            def v4(t):
                                return V(t.h[:].rearrange("p c (ch t) -> p c ch t", t=64), [t.buf])
                            def v4h(t, hb):
                                return V(t.h[hb * 64:(hb + 1) * 64].rearrange("p c (ch t) -> p c ch t", t=64), [t.buf])
                            for hb in range(2):
                                AR_ = ARx[hb]
                                k.stt("dve", V(AR_.h[hb * 64:(hb + 1) * 64, :, :, 0, :], [AR_.buf]), v4h(kn, hb), -1.0, v4h(GP, hb), ALU.mult, ALU.mult)
                                k.tt("dve", V(AR_.h[hb * 64:(hb + 1) * 64, :, :, 1, :], [AR_.buf]), v4h(rT, hb), v4h(G, hb), ALU.mult)
                            prd = rT
                            k.tt("dve", flat(prd), flat(rT), flat(kTl), ALU.mult)
                            for c in range(4):
                                k.ts("dve", prd[:, c, :], prd[:, c, :], pvc(l, "rk", c), ALU.mult)
                            k.tt("dve", flat(bh), flat(kn), flat(a_), ALU.mult)
                            k.tt("dve", flat(bh), flat(bh), flat(GI), ALU.mult)
                            k.tt("dve", flat(kh), flat(kTl), flat(GI), ALU.mult)
                            gcb = V(GC.h[:].unsqueeze(3).broadcast_to([128, 4, NCH, 64]), [GC.buf])
                            bhb, khb, btb, ktb, vbb, prb = bhbr.next(), khbr.next(), btbr.next(), ktbr.next(), vbr.next(), prbr.next()
                            k.tt("dve", v4(btb), v4(bh), gcb, ALU.mult)
                            k.tt("dve", v4(ktb), v4(kh), gcb, ALU.mult)
                            k.copy("act", flat(bhb), flat(bh))
                            k.copy("act", flat(khb), flat(kh))
                            k.copy("act", flat(vbb), flat(vTl))
                            k.copy("act", flat(prb), flat(prd))
                            oTt = oTr2.next()
                            def prep_chunk(ch):
                                csl = slice(ch * 64, (ch + 1) * 64)
                                tok = {}
                                for nm, src in (("b", btb), ("k", ktb), ("v", vbb)):
                                    p = k.ps()
                                    pbb = pbf(p)
                                    for c in range(4):
                                        k.tr(V(pbb.ap[0:64, c * 128:(c + 1) * 128], pbb.bufs), src[:, c, csl], idb[:])
                                    tk = tokr[nm].next()
                                    evac(tk[:], V(pbb.ap[0:64, 0:512], pbb.bufs))
                                    tok[nm] = tk
                                m13, m24 = m13r.next(), m24r.next()
                                for (dst, lh) in ((m13, bhb), (m24, khb)):
                                    for half in range(2):
                                        p = k.ps()
                                        for hh in range(4):
                                            h = half * 4 + hh
                                            c, base = h // 2, 64 * (h % 2)
                                            AR = ARx[h % 2]
                                            k.mm(p[0:64, hh * 128:(hh + 1) * 128], lh[:, c, csl],
                                                 V(AR.h[:, c, ch, :, :].rearrange("p a t -> p (a t)"), [AR.buf]),
                                                 start=(hh == 0), stop=True, skip_group_check=True)
                                        k.tt("dve", dst[:, half * 4:(half + 1) * 4, :, :],
                                             V(p.h[0:64, :].rearrange("p (h a t) -> p h a t", h=4, a=2), [p.buf]),
                                             V(um.h[:, 0:2, :].unsqueeze(1).broadcast_to([64, 4, 2, 64]), [um.buf]), ALU.mult)
                                Mx, Nx, Px, Qx = (sq_[n_].next() for n_ in ("M", "N", "P", "Q"))
                                p = k.ps()
                                for h in range(8):
                                    c, base = h // 2, 64 * (h % 2)
                                    k.mm(p[0:64, h * 64:(h + 1) * 64], ARx[h % 2][:, c, ch, 0, :], bhb[:, c, csl],
                                         start=(h == 0), stop=True, skip_group_check=True)
                                k.tt("dve", Nx[:], V(p.h[0:64, :].rearrange("p (h t) -> p h t", h=8), [p.buf]),
                                     V(um.h[:, 2:3, :].broadcast_to([64, 8, 64]), [um.buf]), ALU.mult)
                                k.copy("act", Mx[:], m13[:, :, 0, :])
                                idb8 = V(idf.h[0:64, 0:64].unsqueeze(1).broadcast_to([64, 8, 64]), [idf.buf])
                                k.tt("dve", Px[:], Mx[:], idb8, ALU.add)
                                k.tt("dve", Qx[:], Nx[:], idb8, ALU.add)
                                for lvl in range(1, 6):
                                    last = lvl == 5
                                    M2_, N2_, P2_, Q2_ = (sq_[n_].next() for n_ in ("M", "N", "P", "Q"))
                                    p = k.ps()
                                    for h in range(8):
                                        k.mm(p[0:64, h * 64:(h + 1) * 64], hv(Nx, h), hv(Mx, h), start=(h == 0), stop=True, skip_group_check=True)
                                    evac(M2_[:], V(p.h[0:64, :].rearrange("p (h t) -> p h t", h=8), [p.buf]))
                                    if not last:
                                        p = k.ps()
                                        for h in range(8):
                                            k.mm(p[0:64, h * 64:(h + 1) * 64], hv(Mx, h), hv(Nx, h), start=(h == 0), stop=True, skip_group_check=True)
                                        evac(N2_[:], V(p.h[0:64, :].rearrange("p (h t) -> p h t", h=8), [p.buf]))
                                    p = k.ps()
                                    for h in range(8):
                                        k.mm(p[0:64, h * 64:(h + 1) * 64], hv(Qx, h), hv(M2_, h), start=(h == 0), stop=True, skip_group_check=True)
                                    k.tt("dve", P2_[:], V(p.h[0:64, :].rearrange("p (h t) -> p h t", h=8), [p.buf]), Px[:], ALU.add)
                                    if not last:
                                        p = k.ps()
                                        for h in range(8):
                                            k.mm(p[0:64, h * 64:(h + 1) * 64], hv(Px, h), hv(N2_, h), start=(h == 0), stop=True, skip_group_check=True)
                                        k.tt("dve", Q2_[:], V(p.h[0:64, :].rearrange("p (h t) -> p h t", h=8), [p.buf]), Qx[:], ALU.add)
                                    Mx, Nx, Px, Qx = M2_, N2_, P2_, Q2_
                                return tok, m13, m24, Px

                            prepared = {0: prep_chunk(0)}
                            for ch in range(NCH):
                                csl = slice(ch * 64, (ch + 1) * 64)
                                if ch + 1 < NCH:
                                    prepared[ch + 1] = prep_chunk(ch + 1)
                                tok, m13, m24, Px = prepared.pop(ch)
                                px = psR()
                                for h in range(8):
                                    c, base = h // 2, 64 * (h % 2)
                                    k.mm(px[0:64, h * 64:(h + 1) * 64], ARx[h % 2][:, c, ch, 0, :], Hb[:, c, :],
                                         start=(h == 0), stop=False, skip_group_check=True)
                                for h in range(8):
                                    k.mm(px[0:64, h * 64:(h + 1) * 64], m24[:, h, 0, :], tok["v"][:, h * 64:(h + 1) * 64],
                                         start=False, stop=True, skip_group_check=True)
                                xs = xsr.next()
                                k.copy("act", xs[:], px[0:64, :])
                                pu = psR()
                                for h in range(8):
                                    k.mm(pu[0:64, h * 64:(h + 1) * 64], hv(Px, h), xs[:, h * 64:(h + 1) * 64],
                                         start=(h == 0), stop=True, skip_group_check=True)
                                us = usr.next()
                                k.copy("act", us[:], pu[0:64, :])
                                po = k.ps()
                                for h in range(8):
                                    c, base = h // 2, 64 * (h % 2)
                                    k.mm(po[0:64, h * 64:(h + 1) * 64], ARx[h % 2][:, c, ch, 1, :], Hb[:, c, :],
                                         start=(h == 0), stop=False, skip_group_check=True)
                                for h in range(8):
                                    k.mm(po[0:64, h * 64:(h + 1) * 64], m13[:, h, 1, :], us[:, h * 64:(h + 1) * 64],
                                         start=False, stop=False, skip_group_check=True)
                                for h in range(8):
                                    k.mm(po[0:64, h * 64:(h + 1) * 64], m24[:, h, 1, :], tok["v"][:, h * 64:(h + 1) * 64],
                                         start=False, stop=True, skip_group_check=True)
                                ph = psR()
                                for c in range(4):
                                    cs = slice(c * 128, (c + 1) * 128)
                                    k.mm(ph[:, cs], tok["b"][:, cs], us[:, cs], start=(c == 0), stop=False, skip_group_check=True)
                                for c in range(4):
                                    cs = slice(c * 128, (c + 1) * 128)
                                    k.mm(ph[:, cs], tok["k"][:, cs], tok["v"][:, cs], start=False, stop=True, skip_group_check=True)
                                Hn = Hr.next()
                                for c in range(4):
                                    for hb in range(2):
                                        ps_ = slice(hb * 64, (hb + 1) * 64)
                                        k.stt("dve", Hn[ps_, c, :], H[ps_, c, :], GC[ps_, c, ch:ch + 1],
                                              ph[ps_, c * 128 + hb * 64:c * 128 + (hb + 1) * 64], ALU.mult, ALU.add)
                                H = Hn
                                Hb = Hbr.next()
                                k.copy("act", Hb[:], Hn[:])
                                osb = osr.next()
                                k.copy("act", osb[:], po[0:64, :])
                                o3 = V(osb.h[:, :].rearrange("p (h v) -> p h v", h=8), [osb.buf])
                                s1, s2, bon = s8r.next(), s8r.next(), s8r.next()
                                k.reduce(s1[:], o3, ALU.add)
                                k.ts("dve", s1[:], s1[:], 1.0 / 64, ALU.mult)
                                ce, sq2 = cer.next(), sqr2.next()
                                ce3 = V(ce.h[:, :].rearrange("p (h v) -> p h v", h=8), [ce.buf])
                                k.tt("dve", ce3, o3, V(s1.h[:, :].unsqueeze(2).broadcast_to([64, 8, 64]), [s1.buf]), ALU.subtract)
                                k.tt("dve", sq2[:], ce[:], ce[:], ALU.mult)
                                k.reduce(s2[:], V(sq2.h[:, :].rearrange("p (h v) -> p h v", h=8), [sq2.buf]), ALU.add)
                                k.act(s2[:], s2[:], AF.Sqrt, bias=epsg[0:64, :], scale=1.0 / 64)
                                k.recip(s2[:], s2[:])
                                k.tt("dve", ce3, ce3, V(s2.h[:, :].unsqueeze(2).broadcast_to([64, 8, 64]), [s2.buf]), ALU.mult)
                                k.tt("dve", ce[:], ce[:], lnw[:], ALU.mult)
                                k.tt("dve", ce[:], ce[:], lnb[:], ALU.add)
                                pb_ = k.ps()
                                for c in range(4):
                                    k.mm(pb_[0:64, 2 * c:2 * c + 2], prb[:, c, csl], ind2[:], start=(c == 0), stop=True, skip_group_check=True)
                                k.copy("act", bon[:], pb_[0:64, 0:8])
                                k.tt("dve", V(sq2.h[:, :].rearrange("p (h v) -> p h v", h=8), [sq2.buf]),
                                     V(tok["v"].h[:, :].rearrange("p (h v) -> p h v", h=8), [tok["v"].buf]),
                                     V(bon.h[:, :].unsqueeze(2).broadcast_to([64, 8, 64]), [bon.buf]), ALU.mult)
                                k.tt("dve", ce[:], ce[:], sq2[:], ALU.add)
                                pg_ = k.ps()
                                k.mm(pg_[0:64, :], gsl[:, 0, csl], g2s[:, 0, :], start=True, stop=False)
                                k.mm(pg_[0:64, :], gsl[0:32, 1, csl], g2s[0:32, 1, :], start=False, stop=True)
                                yb = ybr.next()
                                k.tt("dve", yb[:], pg_[0:64, :], ce[:], ALU.mult)
                                pt = k.ps()
                                ptb = pbf(pt)
                                for c in range(4):
                                    k.tr(V(ptb.ap[:, c * 64:(c + 1) * 64], ptb.bufs), yb[:, c * 128:(c + 1) * 128], idb[0:64, 0:64])
                                k.copy("act", oTt[:, :, csl], V(ptb.ap[:, 0:256].rearrange("p (c t) -> p c t", c=4), ptb.bufs))
                            k.dma("sp", dv(oT["r"], oT["r"].h[:, :, tsl].rearrange("c p t -> p c t")), oTt[:], f"ro{tb % 2}")

            if "M" in phases:
                k.s.barrier()
                with ExitStack() as pes:
                    wb = k.tile([128, 12, D], BF16, "wb", es=pes)
                    wo = k.tile([128, 8, D], BF16, "wo", es=pes)
                    for i in range(3):
                        for hf in range(2):
                            wq[0] += 1
                            k.dma("pool", wb[:, i * 4:(i + 1) * 4, hf * 512:(hf + 1) * 512],
                                  dv(w_branch, w_branch.h[l, i, :, hf * 512:(hf + 1) * 512].rearrange("(kc p) c -> p kc c", p=128)),
                                  f"w{wq[0] % 2}")
                    for hf in range(2):
                        wq[0] += 1
                        k.dma("pool", wo[:, :, hf * 512:(hf + 1) * 512],
                              dv(w_o, w_o.h[l, :, hf * 512:(hf + 1) * 512].rearrange("(kc p) c -> p kc c", p=128)),
                              f"w{wq[0] % 2}")
                    otr = Rot(k, [128, 12, 512], BF16, "ot", 2, es=pes)
                    sgr = Rot(k, [128, 24, 512], BF16, "sgl", 2, es=pes)
                    zTr = Rot(k, [128, 8, 512], BF16, "zT", 2, es=pes)
                    t0r = Rot(k, [128, 512], F32, "t0_", 3, es=pes)
                    xr = Rot(k, [128, D], F32, "xrm", 3, es=pes)
                    for tb in range(lim.get("mtb", T // 512)):
                        ot, sg, zT = otr.next(), sgr.next(), zTr.next()
                        tsl = slice(tb * 512, (tb + 1) * 512)
                        for i, nm in enumerate(("m", "r", "d")):
                            k.dma("sp", ot[:, i * 4:(i + 1) * 4, :], dv(oT[nm], oT[nm].h[:, :, tsl].rearrange("c p t -> p c t")),
                                  f"ml{i}")
                        k.dma("sp", sg[:], dv(sgT, sgT.h[:, :, tsl].rearrange("c p t -> p c t")), "ml3")
                        for cc in range(8):
                            acc = None
                            for i in range(3):
                                p = k.ps()
                                for kc in range(4):
                                    k.mm(p[:], wb[:, i * 4 + kc, cc * 128:(cc + 1) * 128], ot[:, i * 4 + kc, :],
                                         start=(kc == 0), stop=(kc == 3))
                                t0 = t0r.next()
                                k.tt("dve", t0[:], p[:], sg[:, i * 8 + cc, :], ALU.mult)
                                if acc is None:
                                    acc = t0
                                elif i == 1:
                                    k.tt("dve", t0[:], t0[:], acc[:], ALU.add)
                                    acc = t0
                                else:
                                    k.tt("dve", zT[:, cc, :], t0[:], acc[:], ALU.add)
                        for tt in range(4):
                            xt = xr.next()
                            r0 = tb * 512 + tt * 128
                            k.dma("sp", xt[:], xsrc[r0:r0 + 128, :], f"xl{tt % 2}")
                            for nb in range(2):
                                p = k.ps()
                                for kc in range(8):
                                    k.mm(p[:], zT[:, kc, tt * 128:(tt + 1) * 128], wo[:, kc, nb * 512:(nb + 1) * 512],
                                         start=(kc == 0), stop=(kc == 7))
                                k.tt("dve", xt[:, nb * 512:(nb + 1) * 512], p[:], xt[:, nb * 512:(nb + 1) * 512], ALU.add)
                            k.dma("sp", xm_d[r0:r0 + 128, :], xt[:], f"xs{tt % 2}")

            if "F" in phases:
                for b in range(lim.get("nb", NB)):
                    k.s.barrier()
                    with ExitStack() as pes:
                        hT = k.tile([128, 8, S], BF16, "hTf", es=pes)
                        aT = k.tile([128, 22, S], BF16, "aT", es=pes)
                        norm_transpose(pes, xm_d, (norm_ffn, norm_ffn.h[l:l + 1, :]), hT, b * S, S // 128, "f")
                        wgr = Rot(k, [128, 8, 512], BF16, "wg", 2, es=pes)
                        wur = Rot(k, [128, 8, 512], BF16, "wu", 2, es=pes)
                        sgr = Rot(k, [128, 512], F32, "sgf", 3, es=pes)
                        for cb in range(6):
                            n = min(512, DFF - cb * 512)
                            wg, wu = wgr.next(), wur.next()
                            load_w(wg, w_ffn_in.h[l, :, cb * 512:cb * 512 + n], w_ffn_in, 8, n)
                            load_w(wu, w_ffn_in.h[l, :, DFF + cb * 512:DFF + cb * 512 + n], w_ffn_in, 8, n)
                            for c in range(n // 128):
                                for tb in range(S // 512):
                                    pg = gemm_fm(wg, c * 128, 128, hT, tb * 512, 8)
                                    pu = gemm_fm(wu, c * 128, 128, hT, tb * 512, 8)
                                    sg = sgr.next()
                                    k.act(sg[:], pg[:], AF.Silu)
                                    k.tt("dve", aT[:, cb * 4 + c, tb * 512:(tb + 1) * 512], pu[:], sg[:], ALU.mult)
                        wout = k.tile([128, 22, 512], BF16, "wout", es=pes)
                        xr = Rot(k, [128, 512], F32, "xrf", 3, es=pes)
                        xts = {}
                        for nb in range(2):
                            wq[0] += 1
                            k.dma("pool", wout[:, 0:11, :],
                                  dv(w_ffn_out, w_ffn_out.h[l, 0:1408, nb * 512:(nb + 1) * 512].rearrange("(kc p) c -> p kc c", p=128)),
                                  f"w{wq[0] % 2}")
                            wq[0] += 1
                            k.dma("pool", wout[:, 11:22, :],
                                  dv(w_ffn_out, w_ffn_out.h[l, 1408:2816, nb * 512:(nb + 1) * 512].rearrange("(kc p) c -> p kc c", p=128)),
                                  f"w{wq[0] % 2}")
                            for tt in range(S // 128):
                                r0 = b * S + tt * 128
                                xt = xr.next()
                                k.dma("sp", xt[:, 0:512], xm_d[r0:r0 + 128, nb * 512:(nb + 1) * 512], f"xl{tt % 2}")
                                p = k.ps()
                                for c in range(22):
                                    k.mm(p[:], aT[:, c, tt * 128:(tt + 1) * 128], wout[:, c, :], start=(c == 0), stop=(c == 21))
                                k.tt("dve", xt[:, 0:512], p[:], xt[:, 0:512], ALU.add)
                                k.dma("sp", xdst[r0:r0 + 128, nb * 512:(nb + 1) * 512], xt[:, 0:512], f"xs{tt % 2}")
        st = k.emit()
    return nc, st


_CACHE = {}


def kernel(**inputs):
    inp = {k_: np.asarray(v) for k_, v in inputs.items()}
    if "nc" not in _CACHE:
        _CACHE["nc"] = build()[0]
    nc = _CACHE["nc"]
    consts = host_consts(inp)
    in_maps = []
    shared = {}
    for nm in ("w_in", "norm_mix", "norm_ffn", "dsa_kv_up", "w_branch", "w_o", "w_ffn_in", "w_ffn_out",
               "rwkv_w2", "rwkv_a2", "rwkv_g2", "rwkv_va", "rwkv_vb", "rwkv_ln_w", "rwkv_ln_b"):
        shared[nm] = np.ascontiguousarray(inp[nm], dtype=np.float32)
    shared.update(consts)
    for c in range(NCORE):
        m = dict(shared)
        m["x"] = np.ascontiguousarray(inp["x"][c * NB:(c + 1) * NB].reshape(T, D), dtype=np.float32)
        in_maps.append(m)
    res = run_bass_kernel_spmd(nc, in_maps, core_ids=list(range(NCORE)))
    out = np.concatenate([np.asarray(r["y"]).reshape(NB, S, D) for r in res.results], axis=0)
    return out.astype(np.float32)
```
